# Optimizing a Trainium2 kernel written in Bass

```python
import math
import jax, jax.numpy as jnp
from jax import lax
import numpy as np

D_MODEL = 1024
BATCH = 8
SEQ = 2048
DEPTH = 2
DEC_BATCH = 32
DEC_SEQ = 4
PAST_LEN = 16384
PAGE_SIZE = 128

N_A_LAYERS = DEPTH // 2
N_B_LAYERS = DEPTH - N_A_LAYERS

ROPE_THETA = 500000.0
EPS = 1e-6
NEG = -1e30

MLA_HEADS = 16
MLA_NOPE = 64
MLA_ROPE = 32
MLA_V = 64
MLA_Q_LORA = 384
MLA_KV_LORA = 256
MLA_LAT = MLA_KV_LORA + MLA_ROPE
Q_BLOCK = 128

DIL_WINDOWS = (128, 512, 2048)
DIL_RATES = (1, 4, 16)
N_GROUPS = 3
DIL_HEADS = 8
DIL_HEAD_DIM = 128
DIL_ROT = DIL_HEAD_DIM // 4

PEER_HEADS = 8
PEER_NKEYS = 128
PEER_EXPERTS = PEER_NKEYS * PEER_NKEYS
PEER_TOPK = 16
PEER_DKEY = 128
PEER_BLOCK = 128

kernel_name = 'hybrid_mla_dilated_peer_step'


def rms(x):
    xf = x.astype(jnp.float32)
    return (xf * lax.rsqrt(jnp.mean(xf * xf, axis=-1, keepdims=True) + EPS)).astype(x.dtype)


def rope(x, pos):
    half = x.shape[-1] // 2
    inv = ROPE_THETA ** (-jnp.arange(half, dtype=jnp.float32) / half)
    ang = pos.astype(jnp.float32)[:, None] * inv
    ang = ang.reshape(ang.shape[:1] + (1,) * (x.ndim - 3) + (half,))
    cos, sin = jnp.cos(ang), jnp.sin(ang)
    xf = x.astype(jnp.float32)
    x1, x2 = xf[..., :half], xf[..., half:]
    return jnp.concatenate([x1 * cos - x2 * sin, x2 * cos + x1 * sin], axis=-1).astype(x.dtype)


def partial_rope(x, pos):
    return jnp.concatenate([rope(x[..., :DIL_ROT], pos), x[..., DIL_ROT:]], axis=-1)


def modulation(c, w, b):
    return (jax.nn.silu(c) @ w + b)[:, None, :]


def ada(x, shift, scale):
    return rms(x) * (1.0 + scale) + shift


def mla_project(h, pos, w_dq, g_cq, w_uq, w_dkv, g_ckv, g_qn, g_qr, g_kr):
    B, T, _ = h.shape
    cq = rms(h @ w_dq) * g_cq
    q = (cq @ w_uq).reshape(B, T, MLA_HEADS, MLA_NOPE + MLA_ROPE)
    q = jnp.concatenate([rms(q[..., :MLA_NOPE]) * g_qn,
                         rope(rms(q[..., MLA_NOPE:]) * g_qr, pos)], axis=-1)
    kv = h @ w_dkv
    ckv = rms(kv[..., :MLA_KV_LORA]) * g_ckv
    k_pe = rope(rms(kv[..., MLA_KV_LORA:]) * g_kr, pos)
    return q, jnp.concatenate([ckv, k_pe], axis=-1)


def mla_expand(lat, w_uk, w_uv, g_kn):
    ckv, k_pe = lat[..., :MLA_KV_LORA], lat[..., MLA_KV_LORA:]
    k_nope = rms(jnp.einsum('btr,rhd->bthd', ckv, w_uk)) * g_kn
    k_pe = jnp.broadcast_to(k_pe[:, :, None, :], k_nope.shape[:-1] + (MLA_ROPE,))
    v = jnp.einsum('btr,rhd->bthd', ckv, w_uv)
    return jnp.concatenate([k_nope, k_pe], axis=-1), v


def causal_attend(q, k, v, q_pos, k_pos):
    s = jnp.einsum('bqhd,bkhd->bhqk', q, k, preferred_element_type=jnp.float32) * (q.shape[-1] ** -0.5)
    s = jnp.where(k_pos[None, :] <= q_pos[:, None], s, NEG)
    p = jax.nn.softmax(s, axis=-1).astype(v.dtype)
    return jnp.einsum('bhqk,bkhd->bqhd', p, v)


def mla_attend_prompt(q, lat, pos, w_uk, w_uv, g_kn):
    B, S = q.shape[:2]
    k, v = mla_expand(lat, w_uk, w_uv, g_kn)
    nb = S // Q_BLOCK
    qb = jnp.moveaxis(q.reshape(B, nb, Q_BLOCK, MLA_HEADS, q.shape[-1]), 1, 0)
    pb = pos.reshape(nb, Q_BLOCK)
    out = lax.map(lambda a: causal_attend(a[0], k, v, a[1], pos), (qb, pb))
    return jnp.moveaxis(out, 0, 1).reshape(B, S, MLA_HEADS * MLA_V)


def mla_attend_sample(q, lat_new, pool, page_table, w_uk, w_uv, g_kn):
    DB, T = q.shape[:2]
    past = page_table.shape[1] * PAGE_SIZE
    k_pos = jnp.arange(past + T, dtype=jnp.int32)
    q_pos = past + jnp.arange(T, dtype=jnp.int32)

    def one(a):
        pt, qs, ln = a
        lat = jnp.concatenate([pool[pt].reshape(past, MLA_LAT), ln], axis=0)[None]
        k, v = mla_expand(lat, w_uk, w_uv, g_kn)
        return causal_attend(qs[None], k, v, q_pos, k_pos)[0]

    out = lax.map(one, (page_table, q, lat_new))
    return out.reshape(DB, T, MLA_HEADS * MLA_V)


def mla_sublayer(x, c, pos, attend, w_mod, b_mod, proj, w_o):
    shift, scale, gate = jnp.split(modulation(c, w_mod, b_mod), 3, axis=-1)
    q, lat = mla_project(ada(x, shift, scale), pos, *proj)
    return x + gate * (attend(q, lat) @ w_o), lat


def shared_kv(s, c, pos, w_mod, b_mod, w_kv, g_k):
    B, T, _ = s.shape
    shift, scale = jnp.split(modulation(c, w_mod, b_mod), 2, axis=-1)
    kv = (ada(s, shift, scale) @ w_kv).reshape(B, T, 2, N_GROUPS, DIL_HEADS, DIL_HEAD_DIM)
    k = partial_rope(rms(kv[:, :, 0]) * g_k[:, None, :], pos)
    return k, kv[:, :, 1]


def dilated_prompt(q, k, v, dil, span):
    B, S, H, Dh = q.shape
    n_sub = S // dil
    n_blk = -(-n_sub // span)
    tail = n_blk * span - n_sub

    def to_sub(a, lead):
        a = a.reshape(B, n_sub, dil, H, Dh).transpose(0, 2, 1, 3, 4)
        return jnp.pad(a, ((0, 0), (0, 0), (lead, tail), (0, 0), (0, 0)))

    qs = to_sub(q, 0).reshape(B, dil, n_blk, span, H, Dh)
    ks = to_sub(k, span).reshape(B, dil, n_blk + 1, span, H, Dh)
    vs = to_sub(v, span).reshape(B, dil, n_blk + 1, span, H, Dh)
    kb = jnp.concatenate([ks[:, :, :-1], ks[:, :, 1:]], axis=3)
    vb = jnp.concatenate([vs[:, :, :-1], vs[:, :, 1:]], axis=3)
    s = jnp.einsum('brnqhd,brnkhd->brnhqk', qs, kb, preferred_element_type=jnp.float32) * (Dh ** -0.5)
    qi = jnp.arange(span)[:, None] + span
    ki = jnp.arange(2 * span)[None, :]
    band = (qi - ki >= 0) & (qi - ki <= span)
    real = (jnp.arange(n_blk)[:, None, None] > 0) | (ki[None] >= span)
    s = jnp.where((band[None] & real)[:, None], s, NEG)
    m = jnp.max(s, axis=-1)
    p = jnp.exp(s - m[..., None])
    den = jnp.sum(p, axis=-1)
    num = jnp.einsum('brnhqk,brnkhd->brnqhd', p, vb, preferred_element_type=jnp.float32)
    num = num.reshape(B, dil, n_blk * span, H, Dh)[:, :, :n_sub].transpose(0, 2, 1, 3, 4).reshape(B, S, H, Dh)

    def back(a):
        a = jnp.swapaxes(a, -1, -2).reshape(B, dil, n_blk * span, H)[:, :, :n_sub]
        return a.transpose(0, 2, 1, 3).reshape(B, S, H)

    return num, back(m), back(den)


def dilated_sample(q, k_src, v_src, dil, span):
    T, Ls, Dh = q.shape[1], k_src.shape[1], q.shape[-1]
    q_idx = Ls - T + jnp.arange(T)
    kidx = q_idx[:, None] - dil * jnp.arange(span + 1)[None, :]
    valid = kidx >= 0
    kidx = jnp.maximum(kidx, 0)
    kg, vg = k_src[:, kidx], v_src[:, kidx]
    s = jnp.einsum('bqhd,bqjhd->bhqj', q, kg, preferred_element_type=jnp.float32) * (Dh ** -0.5)
    s = jnp.where(valid[None, None], s, NEG)
    m = jnp.max(s, axis=-1)
    p = jnp.exp(s - m[..., None])
    den = jnp.sum(p, axis=-1)
    num = jnp.einsum('bhqj,bqjhd->bqhd', p, vg, preferred_element_type=jnp.float32)
    return num, m.transpose(0, 2, 1), den.transpose(0, 2, 1)


def combine_groups(parts):
    big = parts[0][1]
    for _, m, _ in parts[1:]:
        big = jnp.maximum(big, m)
    num = 0.0
    den = 0.0
    for n_g, m_g, d_g in parts:
        w = jnp.exp(m_g - big)
        num = num + w[..., None] * n_g
        den = den + w * d_g
    return num / den[..., None]


def dil_sublayer(x, c, pos, attend_group, w_mod, b_mod, w_q, g_q, w_o):
    B, T, _ = x.shape
    shift, scale, gate = jnp.split(modulation(c, w_mod, b_mod), 3, axis=-1)
    q = (ada(x, shift, scale) @ w_q).reshape(B, T, N_GROUPS, DIL_HEADS, DIL_HEAD_DIM)
    q = partial_rope(rms(q) * g_q[:, None, :], pos)
    parts = [attend_group(g, q[:, :, g]) for g in range(N_GROUPS)]
    o = combine_groups(parts).astype(x.dtype).reshape(B, T, DIL_HEADS * DIL_HEAD_DIM)
    return x + gate * (o @ w_o)


def peer(h, w_q, subkeys, u_tab, v_tab):
    shape = h.shape
    x = h.reshape(-1, shape[-1])
    n = x.shape[0]
    nb = -(-n // PEER_BLOCK)
    x = jnp.pad(x, ((0, nb * PEER_BLOCK - n), (0, 0)))

    def block(xb):
        q = (xb @ w_q).reshape(-1, PEER_HEADS, 2, PEER_DKEY // 2)
        s = jnp.einsum('thpd,hpkd->thpk', q, subkeys, preferred_element_type=jnp.float32)
        sv, si = lax.top_k(s, PEER_TOPK)
        comb = (sv[:, :, 0, :, None] + sv[:, :, 1, None, :]).reshape(-1, PEER_HEADS, PEER_TOPK * PEER_TOPK)
        cid = (si[:, :, 0, :, None] * PEER_NKEYS + si[:, :, 1, None, :]).reshape(-1, PEER_HEADS, PEER_TOPK * PEER_TOPK)
        top_s, top_j = lax.top_k(comb, PEER_TOPK)
        eid = jnp.take_along_axis(cid, top_j, axis=-1)
        g = jax.nn.softmax(top_s, axis=-1)
        a = jax.nn.gelu(jnp.einsum('td,thkd->thk', xb, u_tab[eid]), approximate=False)
        return jnp.einsum('thk,thkd->td', (g * a).astype(xb.dtype), v_tab[eid])

    y = lax.map(block, x.reshape(nb, PEER_BLOCK, shape[-1]))
    return y.reshape(-1, shape[-1])[:n].reshape(shape)


def peer_sublayer(x, c, w_mod, b_mod, w_q, subkeys, u_tab, v_tab):
    shift, scale, gate = jnp.split(modulation(c, w_mod, b_mod), 3, axis=-1)
    return x + gate * peer(ada(x, shift, scale), w_q, subkeys, u_tab, v_tab)


def setup_inputs(seed: int = 0) -> dict:
    key = jax.random.key(seed)
    keys = list(jax.random.split(key, 48))

    def nrm(shape, scale):
        return jax.random.normal(keys.pop(), shape, jnp.float32) * scale

    def gain(shape):
        return 1.0 + nrm(shape, 0.05)

    D = D_MODEL
    n_pages = PAST_LEN // PAGE_SIZE
    n_used = DEC_BATCH * n_pages
    n_pool = n_used + (n_used + 3) // 4
    page_table = jax.random.permutation(keys.pop(), n_pool)[:n_used].reshape(DEC_BATCH, n_pages).astype(jnp.int32)
    gw = N_GROUPS * DIL_HEADS * DIL_HEAD_DIM
    mod = 0.5 * D ** -0.5
    NA, NB = N_A_LAYERS, N_B_LAYERS
    return {
        'x_prompt': nrm((BATCH, SEQ, D), 1.0),
        'x_sample': nrm((DEC_BATCH, DEC_SEQ, D), 1.0),
        'c_prompt': nrm((BATCH, D), 1.0),
        'c_sample': nrm((DEC_BATCH, D), 1.0),
        'cache_mla': nrm((NA, n_pool, PAGE_SIZE, MLA_LAT), 1.0),
        'cache_dil0': nrm((DEC_BATCH, min(DIL_WINDOWS[0], PAST_LEN), 2, DIL_HEADS, DIL_HEAD_DIM), 1.0),
        'cache_dil1': nrm((DEC_BATCH, min(DIL_WINDOWS[1], PAST_LEN), 2, DIL_HEADS, DIL_HEAD_DIM), 1.0),
        'cache_dil2': nrm((DEC_BATCH, min(DIL_WINDOWS[2], PAST_LEN), 2, DIL_HEADS, DIL_HEAD_DIM), 1.0),
        'page_table': page_table,
        'a_mod_w': nrm((NA, D, 3 * D), mod),
        'a_mod_b': nrm((NA, 3 * D), 0.02),
        'a_w_dq': nrm((NA, D, MLA_Q_LORA), D ** -0.5),
        'a_g_cq': gain((NA, MLA_Q_LORA)),
        'a_w_uq': nrm((NA, MLA_Q_LORA, MLA_HEADS * (MLA_NOPE + MLA_ROPE)), MLA_Q_LORA ** -0.5),
        'a_w_dkv': nrm((NA, D, MLA_LAT), D ** -0.5),
        'a_g_ckv': gain((NA, MLA_KV_LORA)),
        'a_g_qn': gain((NA, MLA_NOPE)),
        'a_g_qr': gain((NA, MLA_ROPE)),
        'a_g_kr': gain((NA, MLA_ROPE)),
        'a_w_uk': nrm((NA, MLA_KV_LORA, MLA_HEADS, MLA_NOPE), MLA_KV_LORA ** -0.5),
        'a_g_kn': gain((NA, MLA_NOPE)),
        'a_w_uv': nrm((NA, MLA_KV_LORA, MLA_HEADS, MLA_V), MLA_KV_LORA ** -0.5),
        'a_w_o': nrm((NA, MLA_HEADS * MLA_V, D), (MLA_HEADS * MLA_V) ** -0.5),
        'kv_mod_w': nrm((D, 2 * D), mod),
        'kv_mod_b': nrm((2 * D,), 0.02),
        'kv_w': nrm((D, 2 * gw), D ** -0.5),
        'kv_g_k': gain((N_GROUPS, DIL_HEAD_DIM)),
        'b_mod_w': nrm((NB, D, 3 * D), mod),
        'b_mod_b': nrm((NB, 3 * D), 0.02),
        'b_w_q': nrm((NB, D, gw), D ** -0.5),
        'b_g_q': gain((NB, N_GROUPS, DIL_HEAD_DIM)),
        'b_w_o': nrm((NB, DIL_HEADS * DIL_HEAD_DIM, D), (DIL_HEADS * DIL_HEAD_DIM) ** -0.5),
        'f_mod_w': nrm((DEPTH, D, 3 * D), mod),
        'f_mod_b': nrm((DEPTH, 3 * D), 0.02),
        'f_w_q': nrm((DEPTH, D, PEER_HEADS * PEER_DKEY), D ** -0.5),
        'f_subkeys': nrm((DEPTH, PEER_HEADS, 2, PEER_NKEYS, PEER_DKEY // 2), (PEER_DKEY // 2) ** -0.5),
        'f_u': nrm((DEPTH, PEER_EXPERTS, D), D ** -0.5),
        'f_v': nrm((DEPTH, PEER_EXPERTS, D), PEER_HEADS ** -0.5),
    }


def reference(x_prompt, x_sample, c_prompt, c_sample, cache_mla, cache_dil0, cache_dil1, cache_dil2,
              page_table, a_mod_w, a_mod_b, a_w_dq, a_g_cq, a_w_uq, a_w_dkv, a_g_ckv, a_g_qn, a_g_qr,
              a_g_kr, a_w_uk, a_g_kn, a_w_uv, a_w_o, kv_mod_w, kv_mod_b, kv_w, kv_g_k, b_mod_w, b_mod_b,
              b_w_q, b_g_q, b_w_o, f_mod_w, f_mod_b, f_w_q, f_subkeys, f_u, f_v):
    caches_dil = (cache_dil0, cache_dil1, cache_dil2)
    seq = x_prompt.shape[1]
    dec = x_sample.shape[1]
    past = page_table.shape[1] * PAGE_SIZE
    pos_p = jnp.arange(seq, dtype=jnp.int32)
    pos_s = past + jnp.arange(dec, dtype=jnp.int32)
    xp, xs = x_prompt, x_sample
    rows_p, rows_s = [], []
    dil_p, dil_s, src_s = [], [], []
    kp = vp = None
    for layer in range(DEPTH):
        if layer < N_A_LAYERS:
            i = layer
            proj = (a_w_dq[i], a_g_cq[i], a_w_uq[i], a_w_dkv[i], a_g_ckv[i], a_g_qn[i], a_g_qr[i], a_g_kr[i])
            expand = (a_w_uk[i], a_w_uv[i], a_g_kn[i])
            xp, lat = mla_sublayer(xp, c_prompt, pos_p,
                                   lambda q, l: mla_attend_prompt(q, l, pos_p, *expand),
                                   a_mod_w[i], a_mod_b[i], proj, a_w_o[i])
            rows_p.append(lat)
            xs, lat = mla_sublayer(xs, c_sample, pos_s,
                                   lambda q, l: mla_attend_sample(q, l, cache_mla[i], page_table, *expand),
                                   a_mod_w[i], a_mod_b[i], proj, a_w_o[i])
            rows_s.append(lat)
        else:
            if layer == N_A_LAYERS:
                kp, vp = shared_kv(xp, c_prompt, pos_p, kv_mod_w, kv_mod_b, kv_w, kv_g_k)
                kn, vn = shared_kv(xs, c_sample, pos_s, kv_mod_w, kv_mod_b, kv_w, kv_g_k)
                for g in range(N_GROUPS):
                    w = DIL_WINDOWS[g]
                    new_p = jnp.stack([kp[:, :, g], vp[:, :, g]], axis=2)
                    full_s = jnp.concatenate([caches_dil[g], jnp.stack([kn[:, :, g], vn[:, :, g]], axis=2)], axis=1)
                    src_s.append(full_s)
                    dil_p.append(new_p[:, -min(w, seq):])
                    dil_s.append(full_s[:, -min(w, past + dec):])
            j = layer - N_A_LAYERS
            att_p = lambda g, qg: dilated_prompt(qg, kp[:, :, g], vp[:, :, g], DIL_RATES[g],
                                                 DIL_WINDOWS[g] // DIL_RATES[g])
            att_s = lambda g, qg: dilated_sample(qg, src_s[g][:, :, 0], src_s[g][:, :, 1], DIL_RATES[g],
                                                 DIL_WINDOWS[g] // DIL_RATES[g])
            xp = dil_sublayer(xp, c_prompt, pos_p, att_p, b_mod_w[j], b_mod_b[j], b_w_q[j], b_g_q[j], b_w_o[j])
            xs = dil_sublayer(xs, c_sample, pos_s, att_s, b_mod_w[j], b_mod_b[j], b_w_q[j], b_g_q[j], b_w_o[j])
        xp = peer_sublayer(xp, c_prompt, f_mod_w[layer], f_mod_b[layer], f_w_q[layer], f_subkeys[layer],
                           f_u[layer], f_v[layer])
        xs = peer_sublayer(xs, c_sample, f_mod_w[layer], f_mod_b[layer], f_w_q[layer], f_subkeys[layer],
                           f_u[layer], f_v[layer])
    mla_p = jnp.stack(rows_p)
    mla_s = jnp.stack(rows_s)
    return (xp, xs, mla_p, mla_s, dil_p[0], dil_s[0], dil_p[1], dil_s[1], dil_p[2], dil_s[2])
```

```python
import contextlib
import numpy as np
import concourse.bass as bass
import concourse.mybir as mybir
from concourse.bass_utils import run_bass_kernel_spmd

F32 = mybir.dt.float32
BF16 = mybir.dt.bfloat16
I32 = mybir.dt.int32
U32 = mybir.dt.uint32
ALU = mybir.AluOpType
AF = mybir.ActivationFunctionType
AX = mybir.AxisListType

NCORES = 8
D = 1024
SEQ = 2048
NT = 16
NS = 16
NTOK = SEQ + NS
EPS = 1e-6
PAST = 16384
MOD_OFF = {'a': 0, 'f0': 3072, 'kv': 6144, 'b': 8192, 'f1': 11264}
MOD_TOT = 14336
SAME_ENG_WINDOW = 3


class Res:
    __slots__ = ('name', 'w', 'r')

    def __init__(self, name):
        self.name = name
        self.w = None
        self.r = {}


class TT(Res):
    __slots__ = ('t',)

    def __init__(self, name, t):
        super().__init__(name)
        self.t = t

    def __getitem__(self, k):
        return self.t[k]


class Sched:
    def __init__(self, nc, stack, M=16):
        self.nc = nc
        self.eng = {'pe': nc.tensor, 'dve': nc.vector, 'act': nc.scalar, 'pool': nc.gpsimd, 'sp': nc.sync}
        self.sem = {e: stack.enter_context(nc.semaphore('s_' + e)) for e in self.eng}
        self.cnt = {e: 0 for e in self.eng}
        self.seen = {e: {} for e in self.eng}
        self.M = M
        self.dsem = {q: [stack.enter_context(nc.semaphore('d_%s%d' % (q, i))) for i in range(M)]
                     for q in ('sp', 'act', 'pool')}
        self.dcnt = {q: 0 for q in self.dsem}
        self.duse = {q: [0] * M for q in self.dsem}
        self.ninstr = 0

    def _wait(self, e, tok):
        key, h, v, owner = tok
        if owner == e:
            if e == 'pe' or v <= self.cnt[e] - SAME_ENG_WINDOW:
                return
        if self.seen[e].get(key, 0) >= v:
            return
        self.eng[e].wait_ge(h, v)
        self.seen[e][key] = v
        self.ninstr += 1

    def _deps(self, e, reads, writes):
        for r in reads:
            if r.w is not None:
                self._wait(e, r.w)
        for w in writes:
            if w.w is not None:
                self._wait(e, w.w)
            for tok in w.r.values():
                self._wait(e, tok)

    def _commit(self, tok, reads, writes):
        for r in reads:
            r.r[tok[0]] = tok
        for w in writes:
            w.w = tok
            w.r = {}

    def op(self, e, fn, reads=(), writes=()):
        self._deps(e, reads, writes)
        ins = fn(self.eng[e])
        self.cnt[e] += 1
        ins.then_inc(self.sem[e], 1)
        self.ninstr += 1
        self._commit((e, self.sem[e], self.cnt[e], e), reads, writes)

    def dma(self, q, fn, reads=(), writes=()):
        slot = self.dcnt[q] % self.M
        self.dcnt[q] += 1
        h = self.dsem[q][slot]
        if self.duse[q][slot] > 0:
            self._wait(q, ((q, slot), h, 16 * self.duse[q][slot], None))
        self._deps(q, reads, writes)
        ins = fn(self.eng[q])
        self.duse[q][slot] += 1
        ins.then_inc(h, 16)
        self.ninstr += 1
        self._commit(((q, slot), h, 16 * self.duse[q][slot], None), reads, writes)

    def barrier(self):
        for e in self.eng:
            for q in self.dsem:
                for slot in range(self.M):
                    if self.duse[q][slot] > 0:
                        self._wait(e, ((q, slot), self.dsem[q][slot], 16 * self.duse[q][slot], None))
            for e2 in self.eng:
                if e2 != e and self.cnt[e2] > 0:
                    self._wait(e, (e2, self.sem[e2], self.cnt[e2], e2))

    def finish(self):
        e = 'sp'
        for q in self.dsem:
            for slot in range(self.M):
                if self.duse[q][slot] > 0:
                    self._wait(e, ((q, slot), self.dsem[q][slot], 16 * self.duse[q][slot], None))
        for e2 in self.eng:
            if e2 != e and self.cnt[e2] > 0:
                self._wait(e, (e2, self.sem[e2], self.cnt[e2], e2))


def gk_g(gk, g):
    class _V:
        pass
    v = TTView(gk, gk.t[:, g, :])
    return v


class TTView:
    def __init__(self, parent, ap):
        self.parent, self.ap = parent, ap

    def __getitem__(self, k):
        return self.ap[k]

    @property
    def w(self):
        return self.parent.w

    @w.setter
    def w(self, v):
        self.parent.w = v

    @property
    def r(self):
        return self.parent.r

    @r.setter
    def r(self, v):
        self.parent.r = v


class Builder:
    def __init__(self, upto='all', debug=False):
        self.upto = upto
        self.debug = debug
        self.nc = bass.Bass("TRN2", target_bir_lowering=False)
        self.stack = contextlib.ExitStack()
        self.S = None
        self.dram = {}

    def din(self, name, shape, dt=F32):
        t = self.nc.dram_tensor(name, list(shape), dt, kind="ExternalInput")
        r = TT(name, t.ap())
        self.dram[name] = r
        return r

    def dout(self, name, shape, dt=F32):
        t = self.nc.dram_tensor(name, list(shape), dt, kind="ExternalOutput")
        r = TT(name, t.ap())
        self.dram[name] = r
        return r

    def dscr(self, name, shape, dt=F32):
        kind = "ExternalOutput" if self.debug else "Internal"
        t = self.nc.dram_tensor(name, list(shape), dt, kind=kind)
        r = TT(name, t.ap())
        self.dram[name] = r
        return r

    def sb(self, name, shape, dt=F32, cm=None):
        self._uid = getattr(self, '_uid', 0) + 1
        name = '%s_%d' % (name, self._uid)
        t = (cm or self.stack).enter_context(self.nc.sbuf_tensor(name, list(shape), dt))
        return TT(name, t)

    def rstd(self, ssq, out, n, inv_count, scratch_eng='act'):
        S = self.S
        (sq_t, sq_ap), (o_t, o_ap) = ssq, out
        S.op('act', lambda e: e.activation(out=o_ap, in_=sq_ap, func=AF.Sqrt, scale=inv_count,
                                           bias=self.eps_t[:n, 0:1]), reads=[sq_t, self.eps_t], writes=[o_t])
        S.op('dve', lambda e: e.reciprocal(out=o_ap, in_=o_ap), reads=[o_t], writes=[o_t])

    def transposes(self, src, src_aps, n, ps_bank, dst, dst_ap, copy_eng='act'):
        S = self.S
        psb = self.ps[:, ps_bank, :].bitcast(BF16)
        wmax = max(a.shape[-1] for a in src_aps)
        for i, a in enumerate(src_aps):
            w = a.shape[-1]
            S.op('pe', lambda e, a=a, i=i, w=w: e.transpose(out=psb[:w, i * 128:i * 128 + n], in_=a,
                                                             identity=self.ident[:n, :n]),
                 reads=[src, self.ident], writes=[self.psr[ps_bank]])
        pv = psb.rearrange("p (k c) -> p k c", c=128)[:wmax, :len(src_aps), :n]
        if copy_eng == 'act':
            S.op('act', lambda e: e.copy(out=dst_ap, in_=pv), reads=[self.psr[ps_bank]], writes=[dst])
        else:
            S.op('dve', lambda e: e.tensor_copy(out=dst_ap, in_=pv), reads=[self.psr[ps_bank]], writes=[dst])

    def _mk_rope(self, t32, t32b):
        S = self.S

        def rope(src_t, src, dst_t, dst, tb, n, H):
            cc = tb[:n, 0:32].unsqueeze(1).broadcast_to([n, H, 32])
            s_lo = tb[:n, 32:48].unsqueeze(1).broadcast_to([n, H, 16])
            s_hi = tb[:n, 48:64].unsqueeze(1).broadcast_to([n, H, 16])
            S.op('dve', lambda e: e.tensor_tensor(out=t32[:n, :H, :], in0=src, in1=cc, op=ALU.mult),
                 reads=[src_t, tb], writes=[t32])
            S.op('dve', lambda e: e.tensor_tensor(out=t32b[:n, :H, 0:16], in0=src[:, :, 16:32], in1=s_lo, op=ALU.mult),
                 reads=[src_t, tb], writes=[t32b])
            S.op('dve', lambda e: e.tensor_tensor(out=t32b[:n, :H, 16:32], in0=src[:, :, 0:16], in1=s_hi, op=ALU.mult),
                 reads=[src_t, tb], writes=[t32b])
            S.op('dve', lambda e: e.tensor_tensor(out=dst, in0=t32[:n, :H, :], in1=t32b[:n, :H, :], op=ALU.add),
                 reads=[t32, t32b], writes=[dst_t])

        self.rope = rope

    def rt_all_tile(self, ti):
        return TTView(self.rt_all, self.rt_all.t[:, ti, :])

    def build(self):
        nc = self.nc
        st = self.stack
        self.S = S = Sched(nc, st)
        st.enter_context(nc.Block())
        dbg = self.debug

        xin = self.din('xin', [NTOK, D])
        cT = self.din('cT', [128, 40])
        rt = self.din('rt', [NTOK, 64])
        identf = self.din('identf', [128, 128])
        modw = {'a': self.din('a_mod_w', [D, 3072]), 'f0': self.din('f_mod_w0', [D, 3072]),
                'kv': self.din('kv_mod_w', [D, 2048]), 'b': self.din('b_mod_w', [D, 3072]),
                'f1': self.din('f_mod_w1', [D, 3072])}
        modb = {'a': self.din('a_mod_b', [1, 3072]), 'f0': self.din('f_mod_b0', [1, 3072]),
                'kv': self.din('kv_mod_b', [1, 2048]), 'b': self.din('b_mod_b', [1, 3072]),
                'f1': self.din('f_mod_b1', [1, 3072])}
        a_w_dq = self.din('a_w_dq', [D, 384])
        a_w_uq = self.din('a_w_uq', [384, 1536])
        a_w_dkv = self.din('a_w_dkv', [D, 288])
        a_w_uk = self.din('a_w_uk', [256, 1024])
        a_w_uv = self.din('a_w_uv', [256, 1024])
        gains = {k: self.din(k, [1, w]) for k, w in
                 [('a_g_cq', 384), ('a_g_ckv', 256), ('a_g_qn', 64), ('a_g_qr', 32), ('a_g_kr', 32), ('a_g_kn', 64)]}

        y_out = self.dout('y', [NTOK, D])
        mla_out = self.dout('mla_rows', [NTOK, 288])

        modv = self.dscr('modv', [5, MOD_TOT])
        qpad_d = self.dscr('qpad_d', [NTOK, 16 * 128], BF16)
        kpad_d = self.dscr('kpad_d', [NTOK, 16 * 128], BF16)
        vaug_d = self.dscr('vaug_d', [NTOK, 16 * 65], BF16)

        self.ps_t = st.enter_context(nc.psum_tensor('ps', [128, 8, 512], F32))
        self.ps = self.ps_t
        self.psr = [Res('psb%d' % i) for i in range(8)]
        self.ident = self.sb('ident', [128, 128], BF16)
        self.eps_t = self.sb('eps', [128, 1], F32)
        S.dma('pool', lambda e: e.dma_start(out=self.ident[:, :], in_=identf[:, :]), reads=[identf], writes=[self.ident])
        S.op('dve', lambda e: e.memset(self.eps_t[:, :], EPS), writes=[self.eps_t])

        with contextlib.ExitStack() as ph:
            cTs = self.sb('cTs', [128, 40], F32, ph)
            silT = self.sb('silT', [128, 40], F32, ph)
            wb = [self.sb('mw%d' % i, [128, 8, 512], F32, ph) for i in range(2)]
            bb = [self.sb('mb%d' % i, [5, 512], F32, ph) for i in range(2)]
            ob = [self.sb('mo%d' % i, [5, 512], F32, ph) for i in range(2)]
            S.dma('sp', lambda e: e.dma_start(out=cTs[:, :], in_=cT[:, :]), reads=[cT], writes=[cTs])
            S.op('act', lambda e: e.activation(out=silT[:, :], in_=cTs[:, :], func=AF.Silu), reads=[cTs], writes=[silT])
            it = 0
            for key in ('a', 'f0', 'kv', 'b', 'f1'):
                W, Bv = modw[key], modb[key]
                N = W.t.shape[1]
                for c in range(N // 512):
                    w_, b_, o_ = wb[it % 2], bb[it % 2], ob[it % 2]
                    pb = it % 2
                    n0 = c * 512
                    S.dma('sp', lambda e, w_=w_, W=W, n0=n0: e.dma_start(
                        out=w_[:, :, :], in_=W[:, n0:n0 + 512].rearrange("(k p) n -> p k n", p=128)),
                        reads=[W], writes=[w_])
                    S.dma('act', lambda e, b_=b_, Bv=Bv, n0=n0: e.dma_start(
                        out=b_[:, :], in_=Bv[0:1, n0:n0 + 512].broadcast_to([5, 512])), reads=[Bv], writes=[b_])
                    for k in range(8):
                        S.op('pe', lambda e, k=k, w_=w_, pb=pb: e.matmul(
                            self.ps[:5, pb, :], lhsT=silT[:, k * 5:(k + 1) * 5], rhs=w_[:, k, :],
                            start=(k == 0), stop=(k == 7)), reads=[silT, w_], writes=[self.psr[pb]])
                    S.op('dve', lambda e, o_=o_, b_=b_, pb=pb: e.tensor_tensor(
                        out=o_[:, :], in0=self.ps[:5, pb, :], in1=b_[:, :], op=ALU.add),
                        reads=[self.psr[pb], b_], writes=[o_])
                    off = MOD_OFF[key] + n0
                    S.dma('sp', lambda e, o_=o_, off=off: e.dma_start(out=modv[:, off:off + 512], in_=o_[:, :]),
                          reads=[o_], writes=[modv])
                    it += 1
            S.barrier()
        if self.upto == 'M':
            return self.finish()

        self.pm = self.sb('pm', [128, 3, D], F32)
        self.sm = self.sb('sm', [128, 3, D], F32)

        def load_mod(key, nparts):
            off = MOD_OFF[key]
            for j in range(nparts):
                S.dma('sp', lambda e, j=j: e.dma_start(
                    out=self.pm[:, j, :], in_=modv[0:1, off + j * D: off + (j + 1) * D].broadcast_to([128, D])),
                    reads=[modv], writes=[self.pm])
                for s in range(4):
                    S.dma('act', lambda e, j=j, s=s: e.dma_start(
                        out=self.sm[4 * s:4 * s + 4, j, :],
                        in_=modv[1 + s:2 + s, off + j * D: off + (j + 1) * D].broadcast_to([4, D])),
                        reads=[modv], writes=[self.sm])
            S.op('dve', lambda e: e.tensor_scalar_add(out=self.pm[:, 1, :], in0=self.pm[:, 1, :], scalar1=1.0),
                 reads=[self.pm], writes=[self.pm])
            S.op('dve', lambda e: e.tensor_scalar_add(out=self.sm[:NS, 1, :], in0=self.sm[:NS, 1, :], scalar1=1.0),
                 reads=[self.sm], writes=[self.sm])

        self.load_mod = load_mod
        self.xb = [self.sb('xb%d' % i, [128, D], F32) for i in range(2)]
        self.junk = self.sb('junk', [128, 1536], F32)
        self.tmp = self.sb('tmpf', [128, D], F32)
        self.hb = self.sb('hb', [128, D], BF16)
        self.hT = self.sb('hT', [128, 8, 128], BF16)
        self.st1 = self.sb('st1', [128, 8], F32)

        def ada_tile(ti, n, row0):
            x = self.xb[ti % 2]
            md = self.pm if n == 128 else self.sm
            S.dma('sp', lambda e: e.dma_start(out=x[:n, :], in_=self.xsrc[row0:row0 + n, :]), reads=[self.xsrc],
                  writes=[x])
            S.op('act', lambda e: e.activation(out=self.junk[:n, :D], in_=x[:n, :], func=AF.Square,
                                               accum_out=self.st1[:n, 0:1]), reads=[x], writes=[self.junk, self.st1])
            self.rstd((self.st1, self.st1[:n, 0:1]), (self.st1, self.st1[:n, 1:2]), n, 1.0 / D)
            S.op('dve', lambda e: e.scalar_tensor_tensor(out=self.tmp[:n, :], in0=x[:n, :], scalar=self.st1[:n, 1:2],
                                                         in1=md[:n, 1, :], op0=ALU.mult, op1=ALU.mult),
                 reads=[x, self.st1, md], writes=[self.tmp])
            S.op('dve', lambda e: e.tensor_tensor(out=self.hb[:n, :], in0=self.tmp[:n, :], in1=md[:n, 0, :],
                                                  op=ALU.add), reads=[self.tmp, md], writes=[self.hb])
            self.transposes(self.hb, [self.hb[:n, k * 128:(k + 1) * 128] for k in range(8)], n, 0, self.hT,
                            self.hT[:, :, :n])
            return x

        self.ada_tile = ada_tile
        self.xsrc = xin
        self.attn = self.sb('attn', [128, NT + 1, D], BF16)
        self.rt_all = self.sb('rt_all', [128, NT + 1, 64], F32)
        S.dma('sp', lambda e: e.dma_start(out=self.rt_all[:, 0:NT, :], in_=rt[0:SEQ, :].rearrange("(t p) c -> p t c", p=128)),
              reads=[rt], writes=[self.rt_all])
        S.dma('sp', lambda e: e.dma_start(out=self.rt_all[:NS, NT, :], in_=rt[SEQ:NTOK, :]), reads=[rt], writes=[self.rt_all])
        self.phA = None
        self.phase_A1(a_w_dq, a_w_uq, a_w_dkv, a_w_uk, a_w_uv, gains, rt, mla_out, qpad_d, kpad_d, vaug_d)
        if self.upto == 'A1':
            return self.finish()
        self.attn_s_d = self.dscr('attn_s_d', [NS, D], BF16)
        maskd_f = self.din('maskd_f', [128, 128])
        self.iota_d = self.din('iota_d', [1, 16])
        mask4_f = self.din('mask4_f', [4, 64])
        cache2d = self.din('cache_mla', [5120, 128 * 288])
        ptT = self.din('ptT', [128, 4], I32)
        a_w_o = self.din('a_w_o', [D, D])
        x1 = self.dscr('x1', [NTOK, D])
        self.phase_A2(qpad_d, kpad_d, vaug_d, maskd_f)
        if self.upto == 'A2':
            self.dbg_attn = self.dscr('dbg_attn', [NTOK, D], BF16)
            for ti in range(NT):
                S.dma('sp', lambda e, ti=ti: e.dma_start(out=self.dbg_attn[ti * 128:(ti + 1) * 128, :],
                                                         in_=self.attn[:, ti, :]), reads=[self.attn],
                      writes=[self.dbg_attn])
            return self.finish()
        self.phase_A3(cache2d, ptT, qpad_d, kpad_d, vaug_d, mask4_f)
        self.phA.close()
        self.phase_oproj(a_w_o, x1)
        if self.upto == 'A':
            return self.finish()
        f_w_q = [self.din('f_w_q%d' % l, [D, D]) for l in range(2)]
        f_subT = [self.din('f_subT%d' % l, [128, 8, 128]) for l in range(2)]
        f_u = [self.din('f_u%d' % l, [16384, D]) for l in range(2)]
        f_v = [self.din('f_v%d' % l, [16384, D]) for l in range(2)]
        x2 = self.dscr('x2', [NTOK, D])
        ub_d = [self.dscr('ub_d%d' % l, [16384, D], BF16) for l in range(2)]
        vb_d = [self.dscr('vb_d%d' % l, [16384, D], BF16) for l in range(2)]
        self.phase_convert([(f_u[0], ub_d[0]), (f_v[0], vb_d[0]), (f_u[1], ub_d[1]), (f_v[1], vb_d[1])])
        f_u, f_v = ub_d, vb_d
        self.xsrc = x1
        self.phase_peer('f0', f_w_q[0], f_subT[0], f_u[0], f_v[0], x2)
        if self.upto == 'F0':
            return self.finish()
        kv_w = self.din('kv_w', [D, 6144])
        kv_g_k = self.din('kv_g_k', [3, 128])
        b_w_q = self.din('b_w_q', [D, 3072])
        b_g_q = self.din('b_g_q', [3, 128])
        b_w_o = self.din('b_w_o', [D, D])
        mask2_f = self.din('mask2_f', [128, 512])
        bd_f = self.din('bd_f', [8, D + 8])
        Ws = (128, 512, 2048)
        cache_dil = [self.din('cache_dil%d' % g, [4, Ws[g], 2, D]) for g in range(3)]
        dil_p = [self.dout('dil%d_p' % g, [Ws[g], 2, D]) for g in range(3)]
        dil_s = [self.dout('dil%d_s' % g, [4, Ws[g], 2, D]) for g in range(3)]
        kd = self.dscr('kd', [3, NTOK, D], BF16)
        vd = self.dscr('vd', [3, NTOK, D], BF16)
        qd = self.dscr('qd', [3, NTOK, D], BF16)
        dacc = self.dscr('dacc', [3, SEQ, 8 * 129])
        x3 = self.dscr('x3', [NTOK, D])
        for g in range(3):
            W = Ws[g]
            for s_ in range(4):
                for r0 in range(0, W - 4, 256):
                    nr = min(256, W - 4 - r0)
                    S.dma('act', lambda e, g=g, s_=s_, r0=r0, nr=nr: e.dma_start(
                        out=dil_s[g][s_, r0:r0 + nr, :, :].rearrange("r t d -> r (t d)"),
                        in_=cache_dil[g][s_, 4 + r0:4 + r0 + nr, :, :].rearrange("r t d -> r (t d)")),
                        reads=[cache_dil[g]], writes=[dil_s[g]])
        self.xsrc = x2
        self.phase_KV(kv_w, kv_g_k, rt, dil_p, dil_s, kd, vd)
        if self.upto == 'KV':
            return self.finish()
        self.phase_B1(b_w_q, b_g_q, qd)
        self.phase_B2(qd, kd, vd, dacc, mask2_f)
        self.phase_B3(qd, cache_dil, dil_s, bd_f)
        self.phase_oproj(b_w_o, x3)
        if self.upto == 'B':
            return self.finish()
        self.xsrc = x3
        self.phase_peer('f1', f_w_q[1], f_subT[1], f_u[1], f_v[1], y_out)
        return self.finish()

    def phase_A1(self, a_w_dq, a_w_uq, a_w_dkv, a_w_uk, a_w_uv, gains, rt, mla_out, qpad_d, kpad_d, vaug_d):
        S = self.S
        ph1 = contextlib.ExitStack()
        self.phA = ph = contextlib.ExitStack()
        wuk = self.sb('wuk', [128, 2, 1024], BF16, ph)
        wuv = self.sb('wuv', [128, 2, 1024], BF16, ph)
        gt = {}
        for k, g in gains.items():
            w = g.t.shape[1]
            gt[k] = self.sb('t_' + k, [128, w], F32, ph)
            S.dma('act', lambda e, k=k, g=g, w=w: e.dma_start(out=gt[k][:, :], in_=g[0:1, :].broadcast_to([128, w])),
                  reads=[g], writes=[gt[k]])
        qf = self.sb('qf', [128, 1536], F32, ph)
        st = self.sb('stA', [128, 96], F32, ph)
        qpad = self.sb('qpad', [128, 16, 128], BF16, ph)
        kpad = self.sb('kpad', [128, 16, 128], BF16, ph)
        vaug = self.sb('vaug', [128, 16, 65], BF16, ph)
        lat = self.sb('lat', [128, 288], F32, ph)
        latb = self.sb('latb', [128, 288], BF16, ph)
        ckvT = self.sb('ckvT', [128, 2, 128], BF16, ph)
        t32 = self.sb('t32', [128, 16, 32], F32, ph)
        t32b = self.sb('t32b', [128, 16, 32], F32, ph)
        wdq = self.sb('wdq', [128, 8, 384], BF16, ph1)
        wuq = self.sb('wuq', [128, 3, 1536], BF16, ph1)
        wdkv = self.sb('wdkv', [128, 8, 288], BF16, ph1)
        for w_, W in ((wdq, a_w_dq), (wuq, a_w_uq), (wdkv, a_w_dkv), (wuk, a_w_uk), (wuv, a_w_uv)):
            S.dma('pool', lambda e, w_=w_, W=W: e.dma_start(
                out=w_[:, :, :], in_=W[:, :].rearrange("(k p) n -> p k n", p=128)), reads=[W], writes=[w_])
        rtb = [self.sb('rtb%d' % i, [128, 64], F32, ph1) for i in range(2)]
        cqb = self.sb('cqb', [128, 384], BF16, ph1)
        cqT = self.sb('cqT', [128, 3, 128], BF16, ph1)
        S.op('pool', lambda e: e.memset(qpad[:, :, :], 0.0), writes=[qpad])
        S.op('pool', lambda e: e.memset(kpad[:, :, :], 0.0), writes=[kpad])
        S.op('pool', lambda e: e.memset(vaug[:, :, :], 1.0), writes=[vaug])
        self.load_mod('a', 3)
        ps, psr = self.ps, self.psr

        def rope(src_t, src, dst_t, dst, tb, n, H):
            cc = tb[:n, 0:32].unsqueeze(1).broadcast_to([n, H, 32])
            s_lo = tb[:n, 32:48].unsqueeze(1).broadcast_to([n, H, 16])
            s_hi = tb[:n, 48:64].unsqueeze(1).broadcast_to([n, H, 16])
            S.op('dve', lambda e: e.tensor_tensor(out=t32[:n, :H, :], in0=src, in1=cc, op=ALU.mult),
                 reads=[src_t, tb], writes=[t32])
            S.op('dve', lambda e: e.tensor_tensor(out=t32b[:n, :H, 0:16], in0=src[:, :, 16:32], in1=s_lo, op=ALU.mult),
                 reads=[src_t, tb], writes=[t32b])
            S.op('dve', lambda e: e.tensor_tensor(out=t32b[:n, :H, 16:32], in0=src[:, :, 0:16], in1=s_hi, op=ALU.mult),
                 reads=[src_t, tb], writes=[t32b])
            S.op('dve', lambda e: e.tensor_tensor(out=dst, in0=t32[:n, :H, :], in1=t32b[:n, :H, :], op=ALU.add),
                 reads=[t32, t32b], writes=[dst_t])

        self.rope = rope

        def expand_tile(n, src_t, src):
            S.op('act', lambda e: e.copy(out=latb[:n, :], in_=src), reads=[src_t], writes=[latb])
            self.transposes(latb, [latb[:n, k * 128:(k + 1) * 128] for k in range(2)], n, 0, ckvT, ckvT[:, :, :n])
            for c in range(2):
                for k in range(2):
                    S.op('pe', lambda e, c=c, k=k: e.matmul(ps[:n, 6 + c, :], lhsT=ckvT[:, k, :n],
                                                            rhs=wuk[:, k, c * 512:(c + 1) * 512],
                                                            start=(k == 0), stop=(k == 1)),
                         reads=[ckvT, wuk], writes=[psr[6 + c]])
            for c in range(2):
                for k in range(2):
                    S.op('pe', lambda e, c=c, k=k: e.matmul(ps[:n, 1 + c, :], lhsT=ckvT[:, k, :n],
                                                            rhs=wuv[:, k, c * 512:(c + 1) * 512],
                                                            start=(k == 0), stop=(k == 1)),
                         reads=[ckvT, wuv], writes=[psr[1 + c]])
            kraw = ps[:n, 6:8, :].rearrange("p b c -> p (b c)")
            k3 = kraw.rearrange("p (h d) -> p h d", d=64)
            S.op('act', lambda e: e.activation(out=self.junk[:n, 0:1024], in_=kraw, func=AF.Square),
                 reads=[psr[6], psr[7]], writes=[self.junk])
            S.op('dve', lambda e: e.tensor_reduce(out=st[:n, 70:86],
                                                  in_=self.junk[:n, 0:1024].rearrange("p (h d) -> p h d", d=64),
                                                  axis=AX.X, op=ALU.add), reads=[self.junk], writes=[st])
            self.rstd((st, st[:n, 70:86]), (st, st[:n, 70:86]), n, 1.0 / 64)
            qk3 = qf[:n, 0:1024].rearrange("p (h d) -> p h d", d=64)
            S.op('dve', lambda e: e.tensor_tensor(out=qk3, in0=k3, in1=st[:n, 70:86].unsqueeze(2).broadcast_to([n, 16, 64]),
                                                  op=ALU.mult), reads=[psr[6], psr[7], st], writes=[qf])
            S.op('dve', lambda e: e.tensor_tensor(out=kpad[:n, :, 0:64], in0=qk3,
                                                  in1=gt['a_g_kn'][:n, :].unsqueeze(1).broadcast_to([n, 16, 64]),
                                                  op=ALU.mult), reads=[qf, gt['a_g_kn']], writes=[kpad])
            S.op('dve', lambda e: e.tensor_copy(out=kpad[:n, :, 64:96],
                                                in_=src[:, 256:288].unsqueeze(1).broadcast_to([n, 16, 32])),
                 reads=[src_t], writes=[kpad])
            S.op('act', lambda e: e.copy(out=vaug[:n, :, 0:64],
                                         in_=ps[:n, 1:3, :].rearrange("p b (h d) -> p (b h) d", d=64)),
                 reads=[psr[1], psr[2]], writes=[vaug])

        self.expand_tile = expand_tile
        self.kpad, self.vaug = kpad, vaug
        for ti in range(NT + 1):
            n = 128 if ti < NT else NS
            row0 = ti * 128
            tb = rtb[ti % 2]
            S.dma('act', lambda e: e.dma_start(out=tb[:n, :], in_=rt[row0:row0 + n, :]), reads=[rt], writes=[tb])
            self.ada_tile(ti, n, row0)
            hT = self.hT
            for k in range(8):
                S.op('pe', lambda e, k=k: e.matmul(ps[:n, 1, 0:384], lhsT=hT[:, k, :n], rhs=wdq[:, k, :],
                                                  start=(k == 0), stop=(k == 7)), reads=[hT, wdq], writes=[psr[1]])
            for k in range(8):
                S.op('pe', lambda e, k=k: e.matmul(ps[:n, 2, 0:288], lhsT=hT[:, k, :n], rhs=wdkv[:, k, :],
                                                  start=(k == 0), stop=(k == 7)), reads=[hT, wdkv], writes=[psr[2]])
            S.op('act', lambda e: e.activation(out=self.junk[:n, 0:384], in_=ps[:n, 1, 0:384], func=AF.Square,
                                               accum_out=st[:n, 0:1]), reads=[psr[1]], writes=[self.junk, st])
            self.rstd((st, st[:n, 0:1]), (st, st[:n, 1:2]), n, 1.0 / 384)
            S.op('dve', lambda e: e.scalar_tensor_tensor(out=cqb[:n, :], in0=ps[:n, 1, 0:384], scalar=st[:n, 1:2],
                                                         in1=gt['a_g_cq'][:n, :], op0=ALU.mult, op1=ALU.mult),
                 reads=[psr[1], st, gt['a_g_cq']], writes=[cqb])
            self.transposes(cqb, [cqb[:n, k * 128:(k + 1) * 128] for k in range(3)], n, 0, cqT, cqT[:, :, :n])
            for c in range(3):
                for k in range(3):
                    S.op('pe', lambda e, c=c, k=k: e.matmul(ps[:n, 3 + c, :], lhsT=cqT[:, k, :n],
                                                            rhs=wuq[:, k, c * 512:(c + 1) * 512],
                                                            start=(k == 0), stop=(k == 2)),
                         reads=[cqT, wuq], writes=[psr[3 + c]])
            qraw = ps[:n, 3:6, :].rearrange("p b c -> p (b c)")
            q3 = qraw.rearrange("p (h d) -> p h d", d=96)
            S.op('act', lambda e: e.activation(out=self.junk[:n, :], in_=qraw, func=AF.Square),
                 reads=[psr[3], psr[4], psr[5]], writes=[self.junk])
            j3 = self.junk[:n, :].rearrange("p (h d) -> p h d", d=96)
            S.op('dve', lambda e: e.tensor_reduce(out=st[:n, 2:18], in_=j3[:, :, 0:64], axis=AX.X, op=ALU.add),
                 reads=[self.junk], writes=[st])
            S.op('dve', lambda e: e.tensor_reduce(out=st[:n, 18:34], in_=j3[:, :, 64:96], axis=AX.X, op=ALU.add),
                 reads=[self.junk], writes=[st])
            self.rstd((st, st[:n, 2:18]), (st, st[:n, 34:50]), n, 1.0 / 64)
            self.rstd((st, st[:n, 18:34]), (st, st[:n, 50:66]), n, 1.0 / 32)
            qf3 = qf[:n, :].rearrange("p (h d) -> p h d", d=96)
            S.op('dve', lambda e: e.tensor_tensor(out=qf3[:, :, 0:64], in0=q3[:, :, 0:64],
                                                  in1=st[:n, 34:50].unsqueeze(2).broadcast_to([n, 16, 64]),
                                                  op=ALU.mult), reads=[psr[3], psr[4], psr[5], st], writes=[qf])
            S.op('dve', lambda e: e.tensor_tensor(out=qpad[:n, :, 0:64], in0=qf3[:, :, 0:64],
                                                  in1=gt['a_g_qn'][:n, :].unsqueeze(1).broadcast_to([n, 16, 64]),
                                                  op=ALU.mult), reads=[qf, gt['a_g_qn']], writes=[qpad])
            S.op('dve', lambda e: e.tensor_tensor(out=qf3[:, :, 64:96], in0=q3[:, :, 64:96],
                                                  in1=st[:n, 50:66].unsqueeze(2).broadcast_to([n, 16, 32]),
                                                  op=ALU.mult), reads=[psr[3], psr[4], psr[5], st], writes=[qf])
            S.op('dve', lambda e: e.tensor_tensor(out=qf3[:, :, 64:96], in0=qf3[:, :, 64:96],
                                                  in1=gt['a_g_qr'][:n, :].unsqueeze(1).broadcast_to([n, 16, 32]),
                                                  op=ALU.mult), reads=[qf, gt['a_g_qr']], writes=[qf])
            rope(qf, qf3[:, :, 64:96], qpad, qpad[:n, :, 64:96], tb, n, 16)
            S.dma('sp', lambda e: e.dma_start(out=qpad_d[row0:row0 + n, :],
                                              in_=qpad[:n, :, :].rearrange("p h d -> p (h d)")),
                  reads=[qpad], writes=[qpad_d])
            S.op('act', lambda e: e.activation(out=self.junk[:n, 0:256], in_=ps[:n, 2, 0:256], func=AF.Square,
                                               accum_out=st[:n, 66:67]), reads=[psr[2]], writes=[self.junk, st])
            S.op('act', lambda e: e.activation(out=self.junk[:n, 256:288], in_=ps[:n, 2, 256:288], func=AF.Square,
                                               accum_out=st[:n, 67:68]), reads=[psr[2]], writes=[self.junk, st])
            self.rstd((st, st[:n, 66:67]), (st, st[:n, 68:69]), n, 1.0 / 256)
            self.rstd((st, st[:n, 67:68]), (st, st[:n, 69:70]), n, 1.0 / 32)
            S.op('dve', lambda e: e.scalar_tensor_tensor(out=lat[:n, 0:256], in0=ps[:n, 2, 0:256], scalar=st[:n, 68:69],
                                                         in1=gt['a_g_ckv'][:n, :], op0=ALU.mult, op1=ALU.mult),
                 reads=[psr[2], st, gt['a_g_ckv']], writes=[lat])
            S.op('dve', lambda e: e.scalar_tensor_tensor(out=qf[:n, 0:32], in0=ps[:n, 2, 256:288], scalar=st[:n, 69:70],
                                                         in1=gt['a_g_kr'][:n, :], op0=ALU.mult, op1=ALU.mult),
                 reads=[psr[2], st, gt['a_g_kr']], writes=[qf])
            rope(qf, qf[:n, 0:32].unsqueeze(1), lat, lat[:n, 256:288].unsqueeze(1), tb, n, 1)
            S.dma('sp', lambda e: e.dma_start(out=mla_out[row0:row0 + n, :], in_=lat[:n, :]), reads=[lat],
                  writes=[mla_out])
            expand_tile(n, lat, lat[:n, :])
            S.dma('sp', lambda e: e.dma_start(out=kpad_d[row0:row0 + n, :],
                                              in_=kpad[:n, :, :].rearrange("p h d -> p (h d)")),
                  reads=[kpad], writes=[kpad_d])
            S.dma('sp', lambda e: e.dma_start(out=vaug_d[row0:row0 + n, :],
                                              in_=vaug[:n, :, :].rearrange("p h d -> p (h d)")),
                  reads=[vaug], writes=[vaug_d])
        S.barrier()
        ph1.close()

    def phase_A2(self, qpad_d, kpad_d, vaug_d, maskd_f):
        S = self.S
        ps, psr = self.ps, self.psr
        ph = contextlib.ExitStack()
        qh = self.sb('qh', [128, 16, 128], BF16, ph)
        kh = self.sb('kh', [128, 16, 128], BF16, ph)
        vh = self.sb('vh', [128, 16, 65], BF16, ph)
        QT = self.sb('QT', [128, 2048], BF16, ph)
        KT = self.sb('KT', [128, 2048], BF16, ph)
        PT = [self.sb('PT%d' % i, [128, 512], BF16, ph) for i in range(4)]
        rec = self.sb('recA2', [128, 2], F32, ph)
        maskd = self.sb('maskd', [128, 128], BF16, ph)
        S.dma('pool', lambda e: e.dma_start(out=maskd[:, :], in_=maskd_f[:, :]), reads=[maskd_f], writes=[maskd])
        scale = 96.0 ** -0.5
        sbanks = [2, 3, 6, 7]
        cnt = 0
        for h in range(16):
            S.dma('sp', lambda e: e.dma_start(
                out=qh[:, :, :], in_=qpad_d[0:SEQ, h * 128:(h + 1) * 128].rearrange("(t p) d -> p t d", p=128)),
                reads=[qpad_d], writes=[qh])
            S.dma('act', lambda e: e.dma_start(
                out=kh[:, :, :], in_=kpad_d[0:SEQ, h * 128:(h + 1) * 128].rearrange("(t p) d -> p t d", p=128)),
                reads=[kpad_d], writes=[kh])
            S.dma('sp', lambda e: e.dma_start(
                out=vh[:, :, :], in_=vaug_d[0:SEQ, h * 65:(h + 1) * 65].rearrange("(t p) d -> p t d", p=128)),
                reads=[vaug_d], writes=[vh])
            for g in range(2):
                self.transposes(qh, [qh[:, 8 * g + j, :] for j in range(8)], 128, g, QT,
                                QT[:, g * 1024:(g + 1) * 1024].rearrange("p (k c) -> p k c", c=128), copy_eng='dve')
            for g in range(2):
                self.transposes(kh, [kh[:, 8 * g + j, :] for j in range(8)], 128, g, KT,
                                KT[:, g * 1024:(g + 1) * 1024].rearrange("p (k c) -> p k c", c=128), copy_eng='act')
            for i in range(16):
                ob = 4 + (i % 2)
                for j0 in range(0, i + 1, 4):
                    jn = min(4, i + 1 - j0)
                    sbk = sbanks[cnt % 4]
                    pt = PT[cnt % 4]
                    cnt += 1
                    for jj in range(jn):
                        j = j0 + jj
                        S.op('pe', lambda e, jj=jj, j=j, sbk=sbk: e.matmul(
                            ps[:, sbk, jj * 128:(jj + 1) * 128], lhsT=KT[:, j * 128:(j + 1) * 128],
                            rhs=QT[:, i * 128:(i + 1) * 128], start=True, stop=True),
                            reads=[KT, QT], writes=[psr[sbk]])
                    S.op('act', lambda e, sbk=sbk, pt=pt, jn=jn: e.activation(
                        out=pt[:, 0:jn * 128], in_=ps[:, sbk, 0:jn * 128], func=AF.Exp, scale=scale),
                        reads=[psr[sbk]], writes=[pt])
                    if j0 + jn - 1 == i:
                        S.op('dve', lambda e, pt=pt, jn=jn: e.tensor_tensor(
                            out=pt[:, (jn - 1) * 128:jn * 128], in0=pt[:, (jn - 1) * 128:jn * 128], in1=maskd[:, :],
                            op=ALU.mult), reads=[pt, maskd], writes=[pt])
                    for jj in range(jn):
                        j = j0 + jj
                        S.op('pe', lambda e, jj=jj, j=j, pt=pt: e.matmul(
                            ps[:, ob, 0:65], lhsT=pt[:, jj * 128:(jj + 1) * 128], rhs=vh[:, j, :],
                            start=(j == 0), stop=(j == i)), reads=[pt, vh], writes=[psr[ob]])
                S.op('dve', lambda e: e.reciprocal(out=rec[:, 0:1], in_=ps[:, ob, 64:65]), reads=[psr[ob]], writes=[rec])
                S.op('dve', lambda e: e.tensor_scalar(out=self.attn[:, i, h * 64:(h + 1) * 64], in0=ps[:, ob, 0:64],
                                                      scalar1=rec[:, 0:1], scalar2=None, op0=ALU.mult),
                     reads=[psr[ob], rec], writes=[self.attn])
        S.barrier()
        ph.close()

    def phase_A3(self, cache2d, ptT, qpad_d, kpad_d, vaug_d, mask4_f):
        S = self.S
        ps, psr = self.ps, self.psr
        ph = contextlib.ExitStack()
        R = 8
        idx = self.sb('pidx', [128, 4], I32, ph)
        pgb = [self.sb('pg%d' % i, [128, R, 288], F32, ph) for i in range(2)]
        kT = self.sb('kTs', [128, 16, 128], BF16, ph)
        QTs = self.sb('QTs', [128, 16, 4], BF16, ph)
        q4 = self.sb('q4', [4, 16, 128], BF16, ph)
        k4 = self.sb('k4', [4, 16, 128], BF16, ph)
        v4 = self.sb('v4', [4, 16, 65], BF16, ph)
        PTs = self.sb('PTs', [128, 64], BF16, ph)
        acc = self.sb('acc', [4, 3, 512], F32, ph)
        mask4 = self.sb('mask4', [4, 64], BF16, ph)
        rec = self.sb('recA3', [4, 16], F32, ph)
        o4 = self.sb('o4', [4, 1024], BF16, ph)
        scale = 96.0 ** -0.5
        S.dma('sp', lambda e: e.dma_start(out=idx[:, :], in_=ptT[:, :]), reads=[ptT], writes=[idx])
        S.dma('pool', lambda e: e.dma_start(out=mask4[:, :], in_=mask4_f[:, :]), reads=[mask4_f], writes=[mask4])
        NBLK = 128 // R
        idxf = self.sb('pidxf', [128, 4], F32, ph)
        io16 = self.sb('io16a', [128, NBLK], F32, ph)
        idxaf = self.sb('pidxaf', [128, 4, NBLK], F32, ph)
        idxall = self.sb('pidxall', [128, 4, NBLK], I32, ph)
        S.dma('sp', lambda e: e.dma_start(out=io16[:, :], in_=self.iota_d[0:1, 0:NBLK].broadcast_to([128, NBLK])),
              reads=[self.iota_d], writes=[io16])
        S.op('dve', lambda e: e.tensor_copy(out=idxf[:, :], in_=idx[:, :]), reads=[idx], writes=[idxf])
        S.op('dve', lambda e: e.tensor_scalar(out=idxf[:, :], in0=idxf[:, :], scalar1=float(NBLK), scalar2=None,
                                              op0=ALU.mult), reads=[idxf], writes=[idxf])
        S.op('dve', lambda e: e.tensor_tensor(out=idxaf[:, :, :], in0=idxf[:, :].unsqueeze(2).broadcast_to([128, 4, NBLK]),
                                              in1=io16[:, :].unsqueeze(1).broadcast_to([128, 4, NBLK]), op=ALU.add),
             reads=[idxf, io16], writes=[idxaf])
        S.op('dve', lambda e: e.tensor_copy(out=idxall[:, :, :], in_=idxaf[:, :, :]), reads=[idxaf], writes=[idxall])
        cblk = cache2d.t.rearrange("n (b e) -> (n b) e", e=R * 288)

        def attend_tile(n, kp_t, kp, va_t, va, masked):
            for g in range(2):
                self.transposes(kp_t, [kp[:, 8 * g + j, :] for j in range(8)], n, g, kT, kT[:, 8 * g:8 * g + 8, :n],
                                copy_eng='dve' if g == 0 else 'act')
            for h in range(16):
                S.op('pe', lambda e, h=h: e.matmul(ps[:n, 3, h * 4:(h + 1) * 4], lhsT=kT[:, h, :n], rhs=QTs[:, h, :],
                                                   start=True, stop=True), reads=[kT, QTs], writes=[psr[3]])
            S.op('act', lambda e: e.activation(out=PTs[:n, :], in_=ps[:n, 3, 0:64], func=AF.Exp, scale=scale),
                 reads=[psr[3]], writes=[PTs])
            if masked:
                S.op('dve', lambda e: e.tensor_tensor(out=PTs[:n, :], in0=PTs[:n, :], in1=mask4[:n, :], op=ALU.mult),
                     reads=[PTs, mask4], writes=[PTs])
            for h in range(16):
                b, c0 = 4 + h // 7, (h % 7) * 65
                S.op('pe', lambda e, h=h, b=b, c0=c0: e.matmul(ps[:4, b, c0:c0 + 65], lhsT=PTs[:n, h * 4:(h + 1) * 4],
                                                               rhs=va[:, h, :], start=True, stop=True),
                     reads=[PTs, va_t], writes=[psr[b]])
            S.op('dve', lambda e: e.tensor_tensor(out=acc[:, :, 0:455], in0=acc[:, :, 0:455], in1=ps[:4, 4:7, 0:455],
                                                  op=ALU.add), reads=[acc, psr[4], psr[5], psr[6]], writes=[acc])

        it = 0
        for s_ in range(4):
            r0 = SEQ + 4 * s_
            S.dma('sp', lambda e: e.dma_start(out=q4[:, :, :], in_=qpad_d[r0:r0 + 4, :].rearrange("p (h d) -> p h d", d=128)),
                  reads=[qpad_d], writes=[q4])
            S.dma('sp', lambda e: e.dma_start(out=k4[:, :, :], in_=kpad_d[r0:r0 + 4, :].rearrange("p (h d) -> p h d", d=128)),
                  reads=[kpad_d], writes=[k4])
            S.dma('sp', lambda e: e.dma_start(out=v4[:, :, :], in_=vaug_d[r0:r0 + 4, :].rearrange("p (h d) -> p h d", d=65)),
                  reads=[vaug_d], writes=[v4])
            for g in range(2):
                self.transposes(q4, [q4[:, 8 * g + j, :] for j in range(8)], 4, g, QTs, QTs[:, 8 * g:8 * g + 8, :],
                                copy_eng='dve')
            S.op('dve', lambda e: e.memset(acc[:, :, :], 0.0), writes=[acc])
            for rb in range(128 // R):
                pg = pgb[it % 2]
                it += 1
                S.dma('pool', lambda e, pg=pg, rb=rb: e.indirect_dma_start(
                    out=pg[:, :, :].rearrange("p r c -> p (r c)"), out_offset=None, in_=cblk,
                    in_offset=bass.IndirectOffsetOnAxis(ap=idxall[:, s_, rb:rb + 1], axis=0)),
                    reads=[cache2d, idxall], writes=[pg])
                for r in range(R):
                    self.expand_tile(128, pg, pg[:, r, :])
                    attend_tile(128, self.kpad, self.kpad[:, :, :], self.vaug, self.vaug[:, :, :], False)
            attend_tile(4, k4, k4[:, :, :], v4, v4[:, :, :], True)
            for b in range(3):
                nh = 7 if b < 2 else 2
                a3 = acc[:, b, 0:nh * 65].rearrange("p (h d) -> p h d", d=65)
                S.op('dve', lambda e, b=b, nh=nh, a3=a3: e.reciprocal(out=rec[:, 7 * b:7 * b + nh], in_=a3[:, :, 64]),
                     reads=[acc], writes=[rec])
                S.op('dve', lambda e, b=b, nh=nh, a3=a3: e.tensor_tensor(
                    out=o4[:, 7 * b * 64:(7 * b + nh) * 64].rearrange("p (h d) -> p h d", d=64), in0=a3[:, :, 0:64],
                    in1=rec[:, 7 * b:7 * b + nh].unsqueeze(2).broadcast_to([4, nh, 64]), op=ALU.mult),
                    reads=[acc, rec], writes=[o4])
            S.dma('sp', lambda e: e.dma_start(out=self.attn_s_d[4 * s_:4 * s_ + 4, :], in_=o4[:, :]), reads=[o4],
                  writes=[self.attn_s_d])
        S.barrier()
        ph.close()

    def phase_oproj(self, w_o_d, xdst):
        S = self.S
        ps, psr = self.ps, self.psr
        ph = contextlib.ExitStack()
        wo = self.sb('wo', [128, 8, D], BF16, ph)
        aT = self.sb('aT', [128, 8, 128], BF16, ph)
        xo = [self.sb('xo%d' % i, [128, D], F32, ph) for i in range(2)]
        S.dma('pool', lambda e: e.dma_start(out=wo[:, :, :], in_=w_o_d[:, :].rearrange("(k p) n -> p k n", p=128)),
              reads=[w_o_d], writes=[wo])
        S.dma('sp', lambda e: e.dma_start(out=self.attn[:NS, NT, :], in_=self.attn_s_d[:, :]), reads=[self.attn_s_d],
              writes=[self.attn])
        for ti in range(NT + 1):
            n = 128 if ti < NT else NS
            row0 = ti * 128
            md = self.pm if n == 128 else self.sm
            x = self.xb[ti % 2]
            S.dma('sp', lambda e: e.dma_start(out=x[:n, :], in_=self.xsrc[row0:row0 + n, :]), reads=[self.xsrc], writes=[x])
            self.transposes(self.attn, [self.attn[:n, ti, k * 128:(k + 1) * 128] for k in range(8)], n, 0, aT,
                            aT[:, :, :n])
            for c in range(2):
                for k in range(8):
                    S.op('pe', lambda e, c=c, k=k: e.matmul(ps[:n, 1 + c, :], lhsT=aT[:, k, :n],
                                                            rhs=wo[:, k, c * 512:(c + 1) * 512], start=(k == 0),
                                                            stop=(k == 7)), reads=[aT, wo], writes=[psr[1 + c]])
            o = xo[ti % 2]
            S.op('dve', lambda e: e.tensor_tensor(out=o[:n, :], in0=ps[:n, 1:3, :].rearrange("p b c -> p (b c)"),
                                                  in1=md[:n, 2, :], op=ALU.mult), reads=[psr[1], psr[2], md], writes=[o])
            S.op('dve', lambda e: e.tensor_tensor(out=o[:n, :], in0=o[:n, :], in1=x[:n, :], op=ALU.add),
                 reads=[o, x], writes=[o])
            S.dma('act', lambda e: e.dma_start(out=xdst[row0:row0 + n, :], in_=o[:n, :]), reads=[o], writes=[xdst])
        S.barrier()
        ph.close()

    def phase_peer(self, key, w_q_d, subT_d, u_d, v_d, xdst):
        S = self.S
        ps, psr = self.ps, self.psr
        ph = contextlib.ExitStack()
        wq = self.sb('pwq', [128, 8, D], BF16, ph)
        skb = self.sb('skb', [128, 8, 256], BF16, ph)
        qT = self.sb('pqT', [128, 8, 128], BF16, ph)
        s_sb = self.sb('s_sb', [128, 16, 128], F32, ph)
        s2 = self.sb('s2', [128, 16, 128], F32, ph)
        sv = self.sb('sv', [128, 16, 16], F32, ph)
        si = self.sb('si', [128, 16, 16], U32, ph)
        sif = self.sb('sif', [128, 16, 16], F32, ph)
        comb = self.sb('comb', [128, 8, 256], F32, ph)
        oh = self.sb('oh', [128, 8, 256], F32, ph)
        ts = self.sb('ts', [128, 8, 16], F32, ph)
        ts2 = self.sb('ts2', [128, 8, 16], F32, ph)
        tj = self.sb('tj', [128, 8, 16], U32, ph)
        tja = self.sb('tja', [128, 8, 16], U32, ph)
        tjb = self.sb('tjb', [128, 8, 16], U32, ph)
        af = self.sb('af', [128, 2, 128], F32, ph)
        ikjk = self.sb('ikjk', [128, 2, 128], F32, ph)
        eid = self.sb('eid', [128, 128], I32, ph)
        gs = self.sb('gs', [128, 16], F32, ph)
        adot = self.sb('adot', [128, 128], F32, ph)
        wgt = self.sb('wgt', [128, 128], BF16, ph)
        iota16 = self.sb('iota16', [128, 16], F32, ph)
        NB = 4
        ub = [self.sb('ub%d' % i, [128, D], BF16, ph) for i in range(NB)]
        vb = [self.sb('vb%d' % i, [128, D], BF16, ph) for i in range(NB)]
        jb = self.sb('jb', [128, D], BF16, ph)
        Dg = self.sb('Dg', [128, 32, 128], BF16, ph)
        xo = self.sb('pxo', [128, D], F32, ph)
        S.dma('pool', lambda e: e.dma_start(out=wq[:, :, :], in_=w_q_d[:, :].rearrange("(k p) n -> p k n", p=128)),
              reads=[w_q_d], writes=[wq])
        S.op('dve', lambda e: e.memset(skb[:, :, :], 0.0), writes=[skb])
        S.dma('pool', lambda e: e.dma_start(out=skb[0:64, :, 0:128], in_=subT_d[0:64, :, :]), reads=[subT_d], writes=[skb])
        S.dma('pool', lambda e: e.dma_start(out=skb[64:128, :, 128:256], in_=subT_d[64:128, :, :]), reads=[subT_d],
              writes=[skb])
        S.dma('sp', lambda e: e.dma_start(out=iota16[:, :], in_=self.iota_d[0:1, :].broadcast_to([128, 16])),
              reads=[self.iota_d], writes=[iota16])
        self.load_mod(key, 3)
        NEG = -1e30
        for ti in range(NT + 1):
            n = 128 if ti < NT else NS
            row0 = ti * 128
            md = self.pm if n == 128 else self.sm
            x = self.ada_tile(ti, n, row0)
            hT, hb = self.hT, self.hb
            for h in range(8):
                for k in range(8):
                    S.op('pe', lambda e, h=h, k=k: e.matmul(ps[:, 1 + h // 4, (h % 4) * 128:(h % 4) * 128 + n],
                                                            lhsT=wq[:, k, h * 128:(h + 1) * 128], rhs=hT[:, k, :n],
                                                            start=(k == 0), stop=(k == 7)),
                         reads=[wq, hT], writes=[psr[1 + h // 4]])
            S.op('act', lambda e: e.copy(out=qT[:, :, :n],
                                         in_=ps[:, 1:3, :].rearrange("p b (h c) -> p (b h) c", c=128)[:, :, :n]),
                 reads=[psr[1], psr[2]], writes=[qT])
            for h in range(8):
                S.op('pe', lambda e, h=h: e.matmul(ps[:n, 3 + h // 2, (h % 2) * 256:(h % 2) * 256 + 256],
                                                   lhsT=qT[:, h, :n], rhs=skb[:, h, :], start=True, stop=True),
                     reads=[qT, skb], writes=[psr[3 + h // 2]])
            S.op('act', lambda e: e.copy(out=s_sb[:n, :, :].rearrange("p g k -> p (g k)"),
                                         in_=ps[:n, 3:7, :].rearrange("p b c -> p (b c)")),
                 reads=[psr[3], psr[4], psr[5], psr[6]], writes=[s_sb])
            for g in range(16):
                S.op('dve', lambda e, g=g: e.max(out=sv[:n, g, 0:8], in_=s_sb[:n, g, :]), reads=[s_sb], writes=[sv])
            for g in range(16):
                S.op('dve', lambda e, g=g: e.max_index(out=si[:n, g, 0:8], in_max=sv[:n, g, 0:8], in_values=s_sb[:n, g, :]),
                     reads=[s_sb, sv], writes=[si])
            for g in range(16):
                S.op('dve', lambda e, g=g: e.match_replace(out=s2[:n, g, :], in_to_replace=sv[:n, g, 0:8],
                                                          in_values=s_sb[:n, g, :], imm_value=NEG),
                     reads=[s_sb, sv], writes=[s2])
            for g in range(16):
                S.op('dve', lambda e, g=g: e.max(out=sv[:n, g, 8:16], in_=s2[:n, g, :]), reads=[s2], writes=[sv])
            for g in range(16):
                S.op('dve', lambda e, g=g: e.max_index(out=si[:n, g, 8:16], in_max=sv[:n, g, 8:16], in_values=s2[:n, g, :]),
                     reads=[s2, sv], writes=[si])
            sv4 = sv[:n, :, :].rearrange("p (h t) k -> p h t k", t=2)
            comb4 = comb[:n, :, :].rearrange("p h (a b) -> p h a b", b=16)
            S.op('dve', lambda e: e.tensor_tensor(out=comb4, in0=sv4[:, :, 0, :].unsqueeze(3).broadcast_to([n, 8, 16, 16]),
                                                  in1=sv4[:, :, 1, :].unsqueeze(2).broadcast_to([n, 8, 16, 16]),
                                                  op=ALU.add), reads=[sv], writes=[comb])
            for h in range(8):
                S.op('dve', lambda e, h=h: e.max(out=ts[:n, h, 0:8], in_=comb[:n, h, :]), reads=[comb], writes=[ts])
            for h in range(8):
                S.op('dve', lambda e, h=h: e.max_index(out=tj[:n, h, 0:8], in_max=ts[:n, h, 0:8], in_values=comb[:n, h, :]),
                     reads=[comb, ts], writes=[tj])
            for h in range(8):
                S.op('dve', lambda e, h=h: e.match_replace(out=oh[:n, h, :], in_to_replace=ts[:n, h, 0:8],
                                                          in_values=comb[:n, h, :], imm_value=NEG),
                     reads=[comb, ts], writes=[oh])
            for h in range(8):
                S.op('dve', lambda e, h=h: e.max(out=ts[:n, h, 8:16], in_=oh[:n, h, :]), reads=[oh], writes=[ts])
            for h in range(8):
                S.op('dve', lambda e, h=h: e.max_index(out=tj[:n, h, 8:16], in_max=ts[:n, h, 8:16], in_values=oh[:n, h, :]),
                     reads=[oh, ts], writes=[tj])
            S.op('dve', lambda e: e.tensor_single_scalar(out=tja[:n, :, :], in_=tj[:n, :, :], scalar=4,
                                                         op=ALU.logical_shift_right), reads=[tj], writes=[tja])
            S.op('dve', lambda e: e.tensor_single_scalar(out=tjb[:n, :, :], in_=tj[:n, :, :], scalar=15,
                                                         op=ALU.bitwise_and), reads=[tj], writes=[tjb])
            S.op('dve', lambda e: e.tensor_copy(out=af[:n, 0, :], in_=tja[:n, :, :].rearrange("p h k -> p (h k)")),
                 reads=[tja], writes=[af])
            S.op('dve', lambda e: e.tensor_copy(out=af[:n, 1, :], in_=tjb[:n, :, :].rearrange("p h k -> p (h k)")),
                 reads=[tjb], writes=[af])
            S.op('dve', lambda e: e.tensor_copy(out=sif[:n, :, :], in_=si[:n, :, :]), reads=[si], writes=[sif])
            sif4 = sif[:n, :, :].rearrange("p (h t) k -> p h t k", t=2)
            io4 = iota16[:n, :].unsqueeze(1).unsqueeze(1).broadcast_to([n, 8, 16, 16])
            for t in range(2):
                a4 = af[:n, t, :].rearrange("p (h k) -> p h k", k=16).unsqueeze(3).broadcast_to([n, 8, 16, 16])
                oh4 = oh[:n, :, :].rearrange("p h (k a) -> p h k a", a=16)
                S.op('dve', lambda e, a4=a4, oh4=oh4: e.tensor_tensor(out=oh4, in0=a4, in1=io4, op=ALU.is_equal),
                     reads=[af, iota16], writes=[oh])
                S.op('dve', lambda e, t=t, oh4=oh4: e.tensor_tensor(
                    out=oh4, in0=oh4, in1=sif4[:, :, t, :].unsqueeze(2).broadcast_to([n, 8, 16, 16]), op=ALU.mult),
                    reads=[oh, sif], writes=[oh])
                S.op('dve', lambda e, t=t, oh4=oh4: e.tensor_reduce(
                    out=ikjk[:n, t, :].rearrange("p (h k) -> p h k", k=16), in_=oh4, axis=AX.X, op=ALU.add),
                    reads=[oh], writes=[ikjk])
            S.op('dve', lambda e: e.scalar_tensor_tensor(out=af[:n, 0, :], in0=ikjk[:n, 0, :], scalar=128.0,
                                                         in1=ikjk[:n, 1, :], op0=ALU.mult, op1=ALU.add),
                 reads=[ikjk], writes=[af])
            S.op('dve', lambda e: e.tensor_copy(out=eid[:n, :], in_=af[:n, 0, :]), reads=[af], writes=[eid])
            S.op('dve', lambda e: e.tensor_tensor(out=ts2[:n, :, :], in0=ts[:n, :, :],
                                                  in1=ts[:n, :, 0:1].broadcast_to([n, 8, 16]), op=ALU.subtract),
                 reads=[ts], writes=[ts2])
            S.op('act', lambda e: e.activation(out=ts2[:n, :, :], in_=ts2[:n, :, :], func=AF.Exp), reads=[ts2], writes=[ts2])
            S.op('dve', lambda e: e.tensor_reduce(out=gs[:n, 0:8], in_=ts2[:n, :, :], axis=AX.X, op=ALU.add),
                 reads=[ts2], writes=[gs])
            S.op('dve', lambda e: e.reciprocal(out=gs[:n, 8:16], in_=gs[:n, 0:8]), reads=[gs], writes=[gs])
            S.op('dve', lambda e: e.tensor_tensor(out=ts2[:n, :, :], in0=ts2[:n, :, :],
                                                  in1=gs[:n, 8:16].unsqueeze(2).broadcast_to([n, 8, 16]), op=ALU.mult),
                 reads=[ts2, gs], writes=[ts2])
            for m in range(128):
                u_ = ub[m % NB]
                S.dma('pool', lambda e, u_=u_, m=m: e.indirect_dma_start(
                    out=u_[:n, :], out_offset=None, in_=u_d[:, :],
                    in_offset=bass.IndirectOffsetOnAxis(ap=eid[:n, m:m + 1], axis=0)), reads=[u_d, eid], writes=[u_])
                S.op('dve', lambda e, u_=u_, m=m: e.scalar_tensor_tensor(
                    out=jb[:n, :], in0=hb[:n, :], scalar=1.0, in1=u_[:n, :], op0=ALU.mult, op1=ALU.mult,
                    accum_out=adot[:n, m:m + 1]), reads=[hb, u_], writes=[jb, adot])
            S.op('act', lambda e: e.activation(out=adot[:n, :], in_=adot[:n, :], func=AF.Gelu), reads=[adot], writes=[adot])
            S.op('dve', lambda e: e.tensor_tensor(out=wgt[:n, :], in0=adot[:n, :],
                                                  in1=ts2[:n, :, :].rearrange("p h k -> p (h k)"), op=ALU.mult),
                 reads=[adot, ts2], writes=[wgt])
            for m in range(128):
                if m % 32 == 0:
                    S.op('dve', lambda e, m=m: e.tensor_tensor(
                        out=Dg[:n, :, :n], in0=self.ident[:n, :n].unsqueeze(1).broadcast_to([n, 32, n]),
                        in1=wgt[:n, m:m + 32].unsqueeze(2).broadcast_to([n, 32, n]), op=ALU.mult),
                        reads=[self.ident, wgt], writes=[Dg])
                v_ = vb[m % NB]
                S.dma('pool', lambda e, v_=v_, m=m: e.indirect_dma_start(
                    out=v_[:n, :], out_offset=None, in_=v_d[:, :],
                    in_offset=bass.IndirectOffsetOnAxis(ap=eid[:n, m:m + 1], axis=0)), reads=[v_d, eid], writes=[v_])
                for c in range(2):
                    S.op('pe', lambda e, v_=v_, m=m, c=c: e.matmul(
                        ps[:n, 1 + c, :], lhsT=Dg[:n, m % 32, :n], rhs=v_[:n, c * 512:(c + 1) * 512],
                        start=(m == 0), stop=(m == 127)), reads=[Dg, v_], writes=[psr[1 + c]])
            S.op('dve', lambda e: e.tensor_tensor(out=xo[:n, :], in0=ps[:n, 1:3, :].rearrange("p b c -> p (b c)"),
                                                  in1=md[:n, 2, :], op=ALU.mult), reads=[psr[1], psr[2], md], writes=[xo])
            S.op('dve', lambda e: e.tensor_tensor(out=xo[:n, :], in0=xo[:n, :], in1=x[:n, :], op=ALU.add),
                 reads=[xo, x], writes=[xo])
            S.dma('act', lambda e: e.dma_start(out=xdst[row0:row0 + n, :], in_=xo[:n, :]), reads=[xo], writes=[xdst])
        S.barrier()
        ph.close()

    def dil_norm_rope(self, raw, raw_res, gain, tb, n, dst_t, dst):
        S = self.S
        st, kf, tb_t = self.stD, self.kfD, self.rt_all
        S.op('act', lambda e: e.activation(out=self.junk[:n, 0:1024], in_=raw, func=AF.Square), reads=raw_res,
             writes=[self.junk])
        S.op('dve', lambda e: e.tensor_reduce(out=st[:n, 0:8], in_=self.junk[:n, 0:1024].rearrange("p (h d) -> p h d", d=128),
                                              axis=AX.X, op=ALU.add), reads=[self.junk], writes=[st])
        self.rstd((st, st[:n, 0:8]), (st, st[:n, 8:16]), n, 1.0 / 128)
        r3 = raw.rearrange("p (h d) -> p h d", d=128)
        k3 = kf[:n, :].rearrange("p (h d) -> p h d", d=128)
        d3 = dst.rearrange("p (h d) -> p h d", d=128)
        S.op('dve', lambda e: e.tensor_tensor(out=k3, in0=r3, in1=st[:n, 8:16].unsqueeze(2).broadcast_to([n, 8, 128]),
                                              op=ALU.mult), reads=list(raw_res) + [st], writes=[kf])
        S.op('dve', lambda e: e.tensor_tensor(out=d3, in0=k3, in1=gain[:n, :].unsqueeze(1).broadcast_to([n, 8, 128]),
                                              op=ALU.mult), reads=[kf, gain], writes=[dst_t])
        self.rope(dst_t, d3[:, :, 0:32], dst_t, d3[:, :, 0:32], tb, n, 8)

    def phase_KV(self, kv_w, kv_g_k, rt, dil_p, dil_s, kd, vd):
        S = self.S
        ps, psr = self.ps, self.psr
        ph = contextlib.ExitStack()
        self.stD = self.sb('stD', [128, 16], F32, ph)
        self.kfD = self.sb('kfD', [128, D], F32, ph)
        t32 = self.sb('t32d', [128, 16, 32], F32, ph)
        t32b = self.sb('t32bd', [128, 16, 32], F32, ph)
        self._mk_rope(t32, t32b)
        gk = self.sb('gk', [128, 3, 128], F32, ph)
        for g in range(3):
            S.dma('sp', lambda e, g=g: e.dma_start(out=gk[:, g, :], in_=kv_g_k[g:g + 1, :].broadcast_to([128, 128])),
                  reads=[kv_g_k], writes=[gk])
        wkv = [self.sb('wkv%d' % i, [128, 8, 1024], BF16, ph) for i in range(2)]
        of = [self.sb('kvof%d' % i, [128, D], F32, ph) for i in range(2)]
        ob = [self.sb('kvob%d' % i, [128, D], BF16, ph) for i in range(2)]
        hTall = self.attn.t[:, :, :].rearrange("p t d -> p (t d)").rearrange("p (k c) -> p k c", k=8)
        self.load_mod('kv', 2)
        for ti in range(NT + 1):
            n = 128 if ti < NT else NS
            row0 = ti * 128
            self.ada_tile(ti, n, row0)
            S.op('act', lambda e: e.copy(out=hTall[:, :, row0:row0 + n], in_=self.hT[:, :, :n]), reads=[self.hT],
                 writes=[self.attn])
        it = 0
        for cg in range(6):
            two, g = cg // 3, cg % 3
            W = (128, 512, 2048)[g]
            w_ = wkv[cg % 2]
            S.dma('pool', lambda e, w_=w_: e.dma_start(
                out=w_[:, :, :], in_=kv_w[:, cg * 1024:(cg + 1) * 1024].rearrange("(k p) n -> p k n", p=128)),
                reads=[kv_w], writes=[w_])
            for ti in range(NT + 1):
                n = 128 if ti < NT else NS
                row0 = ti * 128
                b0 = 1 + 2 * (it % 2)
                o_f, o_b = of[it % 2], ob[it % 2]
                it += 1
                for c in range(2):
                    for k in range(8):
                        S.op('pe', lambda e, c=c, k=k: e.matmul(ps[:n, b0 + c, :], lhsT=hTall[:, k, row0:row0 + n],
                                                                rhs=w_[:, k, c * 512:(c + 1) * 512], start=(k == 0),
                                                                stop=(k == 7)), reads=[self.attn, w_], writes=[psr[b0 + c]])
                raw = ps[:n, b0:b0 + 2, :].rearrange("p b c -> p (b c)")
                if two == 0:
                    self.dil_norm_rope(raw, [psr[b0], psr[b0 + 1]], gk_g(gk, g), self.rt_all_tile(ti), n, o_f, o_f[:n, :])
                else:
                    S.op('act', lambda e: e.copy(out=o_f[:n, :], in_=raw), reads=[psr[b0], psr[b0 + 1]], writes=[o_f])
                S.op('act', lambda e: e.copy(out=o_b[:n, :], in_=o_f[:n, :]), reads=[o_f], writes=[o_b])
                dst_b = kd if two == 0 else vd
                S.dma('sp', lambda e: e.dma_start(out=dst_b[g, row0:row0 + n, :], in_=o_b[:n, :]), reads=[o_b],
                      writes=[dst_b])
                if ti < NT:
                    lo = SEQ - W
                    if row0 >= lo:
                        S.dma('act', lambda e: e.dma_start(out=dil_p[g][row0 - lo:row0 - lo + n, two, :], in_=o_f[:n, :]),
                              reads=[o_f], writes=[dil_p[g]])
                else:
                    for s_ in range(4):
                        S.dma('act', lambda e, s_=s_: e.dma_start(out=dil_s[g][s_, W - 4:W, two, :],
                                                                  in_=o_f[4 * s_:4 * s_ + 4, :]), reads=[o_f],
                              writes=[dil_s[g]])
        S.barrier()
        ph.close()

    def phase_B1(self, b_w_q, b_g_q, qd):
        S = self.S
        ps, psr = self.ps, self.psr
        ph = contextlib.ExitStack()
        self.stD = self.sb('stD1', [128, 16], F32, ph)
        self.kfD = self.sb('kfD1', [128, D], F32, ph)
        t32 = self.sb('t32e', [128, 16, 32], F32, ph)
        t32b = self.sb('t32be', [128, 16, 32], F32, ph)
        self._mk_rope(t32, t32b)
        gq = self.sb('gq', [128, 3, 128], F32, ph)
        for g in range(3):
            S.dma('sp', lambda e, g=g: e.dma_start(out=gq[:, g, :], in_=b_g_q[g:g + 1, :].broadcast_to([128, 128])),
                  reads=[b_g_q], writes=[gq])
        wq = self.sb('bwq', [128, 8, 3072], BF16, ph)
        of = [self.sb('qof%d' % i, [128, D], F32, ph) for i in range(2)]
        ob = [self.sb('qob%d' % i, [128, D], BF16, ph) for i in range(2)]
        for c in range(3):
            S.dma('pool', lambda e, c=c: e.dma_start(
                out=wq[:, :, c * 1024:(c + 1) * 1024],
                in_=b_w_q[:, c * 1024:(c + 1) * 1024].rearrange("(k p) n -> p k n", p=128)), reads=[b_w_q], writes=[wq])
        self.load_mod('b', 3)
        it = 0
        for ti in range(NT + 1):
            n = 128 if ti < NT else NS
            row0 = ti * 128
            self.ada_tile(ti, n, row0)
            for c in range(6):
                for k in range(8):
                    S.op('pe', lambda e, c=c, k=k: e.matmul(ps[:n, 1 + c, :], lhsT=self.hT[:, k, :n],
                                                            rhs=wq[:, k, c * 512:(c + 1) * 512], start=(k == 0),
                                                            stop=(k == 7)), reads=[self.hT, wq], writes=[psr[1 + c]])
            for g in range(3):
                o_f, o_b = of[it % 2], ob[it % 2]
                it += 1
                raw = ps[:n, 1 + 2 * g:3 + 2 * g, :].rearrange("p b c -> p (b c)")
                self.dil_norm_rope(raw, [psr[1 + 2 * g], psr[2 + 2 * g]], gk_g(gq, g), self.rt_all_tile(ti), n, o_f,
                                   o_f[:n, :])
                S.op('act', lambda e: e.copy(out=o_b[:n, :], in_=o_f[:n, :]), reads=[o_f], writes=[o_b])
                S.dma('sp', lambda e, g=g: e.dma_start(out=qd[g, row0:row0 + n, :], in_=o_b[:n, :]), reads=[o_b], writes=[qd])
        S.barrier()
        ph.close()

    def phase_B2(self, qd, kd, vd, dacc, mask2_f):
        S = self.S
        ps, psr = self.ps, self.psr
        ph = contextlib.ExitStack()
        qt = self.sb('dq', [128, 8, 128], BF16, ph)
        kt = [self.sb('dk%d' % i, [128, 8, 128], BF16, ph) for i in range(2)]
        vt = [self.sb('dv%d' % i, [128, 8, 129], BF16, ph) for i in range(2)]
        QT = self.sb('dQT', [128, 8, 128], BF16, ph)
        KT = [self.sb('dKT%d' % i, [128, 8, 128], BF16, ph) for i in range(2)]
        PT = [self.sb('dPT%d' % i, [128, 512], BF16, ph) for i in range(2)]
        mask2 = self.sb('mask2', [128, 512], BF16, ph)
        oacc = [self.sb('doa%d' % i, [128, 8, 129], F32, ph) for i in range(2)]
        S.dma('pool', lambda e: e.dma_start(out=mask2[:, :], in_=mask2_f[:, :]), reads=[mask2_f], writes=[mask2])
        for i in range(2):
            S.op('pool', lambda e, i=i: e.memset(vt[i][:, :, :], 1.0), writes=[vt[i]])
        scale = 128.0 ** -0.5
        cnt = 0
        tcount = 0
        for g in range(3):
            dil = (1, 4, 16)[g]
            nblk = SEQ // dil // 128
            for r in range(dil):
                for I in range(nblk):
                    cur = tcount % 2
                    prv = 1 - cur
                    tcount += 1
                    t0 = r + dil * 128 * I
                    rows = lambda T: T[g, t0:t0 + dil * 127 + 1:dil, :].rearrange("p (h d) -> p h d", d=128)
                    S.dma('sp', lambda e: e.dma_start(out=qt[:, :, :], in_=rows(qd)), reads=[qd], writes=[qt])
                    S.dma('act', lambda e: e.dma_start(out=kt[cur][:, :, :], in_=rows(kd)), reads=[kd], writes=[kt[cur]])
                    S.dma('sp', lambda e: e.dma_start(out=vt[cur][:, :, 0:128], in_=rows(vd)), reads=[vd], writes=[vt[cur]])
                    self.transposes(qt, [qt[:, h, :] for h in range(8)], 128, 0, QT, QT[:, :, :], copy_eng='dve')
                    self.transposes(kt[cur], [kt[cur][:, h, :] for h in range(8)], 128, 0, KT[cur], KT[cur][:, :, :],
                                    copy_eng='act')
                    oa = oacc[cur]
                    for hp in range(4):
                        sb_ = 1 + (cnt % 2)
                        pt = PT[cnt % 2]
                        cnt += 1
                        for hh in range(2):
                            h = 2 * hp + hh
                            if I > 0:
                                S.op('pe', lambda e, h=h, hh=hh: e.matmul(ps[:, sb_, hh * 256:hh * 256 + 128],
                                                                         lhsT=KT[prv][:, h, :], rhs=QT[:, h, :],
                                                                         start=True, stop=True),
                                     reads=[KT[prv], QT], writes=[psr[sb_]])
                            S.op('pe', lambda e, h=h, hh=hh: e.matmul(ps[:, sb_, hh * 256 + 128:hh * 256 + 256],
                                                                     lhsT=KT[cur][:, h, :], rhs=QT[:, h, :],
                                                                     start=True, stop=True),
                                 reads=[KT[cur], QT], writes=[psr[sb_]])
                        if I > 0:
                            S.op('act', lambda e: e.activation(out=pt[:, :], in_=ps[:, sb_, :], func=AF.Exp, scale=scale),
                                 reads=[psr[sb_]], writes=[pt])
                        else:
                            for hh in range(2):
                                S.op('act', lambda e, hh=hh: e.activation(
                                    out=pt[:, hh * 256 + 128:hh * 256 + 256], in_=ps[:, sb_, hh * 256 + 128:hh * 256 + 256],
                                    func=AF.Exp, scale=scale), reads=[psr[sb_]], writes=[pt])
                        S.op('dve', lambda e: e.tensor_tensor(out=pt[:, :], in0=pt[:, :], in1=mask2[:, :], op=ALU.mult),
                             reads=[pt, mask2], writes=[pt])
                        for hh in range(2):
                            h = 2 * hp + hh
                            ob_, c0 = 4 + h // 3, (h % 3) * 129
                            if I > 0:
                                S.op('pe', lambda e, h=h, hh=hh, ob_=ob_, c0=c0: e.matmul(
                                    ps[:, ob_, c0:c0 + 129], lhsT=pt[:, hh * 256:hh * 256 + 128], rhs=vt[prv][:, h, :],
                                    start=True, stop=False), reads=[pt, vt[prv]], writes=[psr[ob_]])
                            S.op('pe', lambda e, h=h, hh=hh, ob_=ob_, c0=c0: e.matmul(
                                ps[:, ob_, c0:c0 + 129], lhsT=pt[:, hh * 256 + 128:hh * 256 + 256], rhs=vt[cur][:, h, :],
                                start=(I == 0), stop=True), reads=[pt, vt[cur]], writes=[psr[ob_]])
                    for b in range(3):
                        nh = 3 if b < 2 else 2
                        S.op('dve', lambda e, b=b, nh=nh: e.tensor_copy(
                            out=oa[:, 3 * b:3 * b + nh, :].rearrange("p h d -> p (h d)"), in_=ps[:, 4 + b, 0:nh * 129]),
                            reads=[psr[4 + b]], writes=[oa])
                    S.dma('act', lambda e: e.dma_start(out=dacc[g, t0:t0 + dil * 127 + 1:dil, :],
                                                       in_=oa[:, :, :].rearrange("p h d -> p (h d)")), reads=[oa],
                          writes=[dacc])
        cb = [self.sb('dcb%d' % i, [128, 3, 8 * 129], F32, ph) for i in range(2)]
        rc = self.sb('drc', [128, 8], F32, ph)
        for ti in range(NT):
            c_ = cb[ti % 2]
            for g in range(3):
                S.dma('sp', lambda e, g=g: e.dma_start(out=c_[:, g, :], in_=dacc[g, ti * 128:(ti + 1) * 128, :]),
                      reads=[dacc], writes=[c_])
            S.op('dve', lambda e: e.tensor_tensor(out=c_[:, 0, :], in0=c_[:, 0, :], in1=c_[:, 1, :], op=ALU.add),
                 reads=[c_], writes=[c_])
            S.op('dve', lambda e: e.tensor_tensor(out=c_[:, 0, :], in0=c_[:, 0, :], in1=c_[:, 2, :], op=ALU.add),
                 reads=[c_], writes=[c_])
            c3 = c_[:, 0, :].rearrange("p (h d) -> p h d", d=129)
            S.op('dve', lambda e: e.reciprocal(out=rc[:, :], in_=c3[:, :, 128]), reads=[c_], writes=[rc])
            S.op('dve', lambda e: e.tensor_tensor(out=self.attn[:, ti, :].rearrange("p (h d) -> p h d", d=128),
                                                  in0=c3[:, :, 0:128], in1=rc[:, :].unsqueeze(2).broadcast_to([128, 8, 128]),
                                                  op=ALU.mult), reads=[c_, rc], writes=[self.attn])
        S.barrier()
        ph.close()

    def phase_B3(self, qd, cache_dil, dil_s, bd_f):
        S = self.S
        ps, psr = self.ps, self.psr
        ph = contextlib.ExitStack()
        kc = [self.sb('skc%d' % i, [128, D], F32, ph) for i in range(2)]
        vc = [self.sb('svc%d' % i, [128, D + 8], F32, ph) for i in range(2)]
        kn = [self.sb('skn%d' % i, [4, D], F32, ph) for i in range(2)]
        vn = [self.sb('svn%d' % i, [4, D + 8], F32, ph) for i in range(2)]
        qb = [self.sb('sqb%d' % i, [128, D], BF16, ph) for i in range(2)]
        pr = self.sb('spr', [128, D], F32, ph)
        sc = self.sb('ssc', [128, 16], F32, ph)
        P = [self.sb('sP%d' % i, [128, 16], F32, ph) for i in range(2)]
        o8 = self.sb('so8', [8, D + 8], F32, ph)
        bd = self.sb('sbd', [8, D + 8], F32, ph)
        ones8 = self.sb('sones', [8, 1], F32, ph)
        row = self.sb('srow', [1, D + 8], F32, ph)
        rrec = self.sb('srrec', [1, 8], F32, ph)
        orow = self.sb('sorow', [1, D], BF16, ph)
        S.dma('sp', lambda e: e.dma_start(out=bd[:, :], in_=bd_f[:, :]), reads=[bd_f], writes=[bd])
        S.op('dve', lambda e: e.memset(ones8[:, :], 1.0), writes=[ones8])
        for i in range(2):
            S.op('dve', lambda e, i=i: e.memset(vc[i][:, D:D + 8], 1.0), writes=[vc[i]])
            S.op('dve', lambda e, i=i: e.memset(vn[i][:, D:D + 8], 1.0), writes=[vn[i]])
        scale = 128.0 ** -0.5
        it = 0
        for s_ in range(4):
            for t in range(4):
                tok = SEQ + 4 * s_ + t
                nmm = 0
                for g in range(3):
                    dil = (1, 4, 16)[g]
                    W = (128, 512, 2048)[g]
                    Mc = 128 if dil > 1 else 128 - t
                    n0, nn = (t, 1) if dil > 1 else (0, t + 1)
                    b = it % 2
                    it += 1
                    kc_, vc_, kn_, vn_, qb_, P_ = kc[b], vc[b], kn[b], vn[b], qb[b], P[b]
                    cd = cache_dil[g]
                    S.dma('sp', lambda e: e.dma_start(out=kc_[:Mc, :], in_=cd[s_, t:t + dil * (Mc - 1) + 1:dil, 0, :]),
                          reads=[cd], writes=[kc_])
                    S.dma('act', lambda e: e.dma_start(out=vc_[:Mc, 0:D], in_=cd[s_, t:t + dil * (Mc - 1) + 1:dil, 1, :]),
                          reads=[cd], writes=[vc_])
                    S.dma('sp', lambda e: e.dma_start(out=kn_[:nn, :], in_=dil_s[g][s_, W - 4 + n0:W - 4 + n0 + nn, 0, :]),
                          reads=[dil_s[g]], writes=[kn_])
                    S.dma('act', lambda e: e.dma_start(out=vn_[:nn, 0:D], in_=dil_s[g][s_, W - 4 + n0:W - 4 + n0 + nn, 1, :]),
                          reads=[dil_s[g]], writes=[vn_])
                    S.dma('sp', lambda e: e.dma_start(out=qb_[:, :], in_=qd[g, tok:tok + 1, :].broadcast_to([128, D])),
                          reads=[qd], writes=[qb_])
                    for (src, m, col) in ((kc_, Mc, 0), (kn_, nn, 8)):
                        S.op('dve', lambda e, src=src, m=m: e.tensor_tensor(out=pr[:m, :], in0=src[:m, :], in1=qb_[:m, :],
                                                                            op=ALU.mult), reads=[src, qb_], writes=[pr])
                        S.op('dve', lambda e, m=m, col=col: e.tensor_reduce(
                            out=sc[:m, col:col + 8], in_=pr[:m, :].rearrange("p (h d) -> p h d", d=128), axis=AX.X,
                            op=ALU.add), reads=[pr], writes=[sc])
                        S.op('act', lambda e, m=m, col=col: e.activation(out=P_[:m, col:col + 8], in_=sc[:m, col:col + 8],
                                                                        func=AF.Exp, scale=scale), reads=[sc], writes=[P_])
                    for (pv, m, col) in ((vc_, Mc, 0), (vn_, nn, 8)):
                        for c, (c0, cw) in enumerate(((0, 512), (512, 512), (1024, 8))):
                            S.op('pe', lambda e, pv=pv, m=m, col=col, c=c, c0=c0, cw=cw: e.matmul(
                                ps[:8, 1 + c, 0:cw], lhsT=P_[:m, col:col + 8], rhs=pv[:m, c0:c0 + cw],
                                start=(nmm == 0), stop=(nmm == 5)), reads=[P_, pv], writes=[psr[1 + c]])
                        nmm += 1
                S.op('dve', lambda e: e.tensor_tensor(out=o8[:, 0:1024], in0=ps[:8, 1:3, :].rearrange("p b c -> p (b c)"),
                                                      in1=bd[:, 0:1024], op=ALU.mult), reads=[psr[1], psr[2], bd], writes=[o8])
                S.op('dve', lambda e: e.tensor_tensor(out=o8[:, 1024:1032], in0=ps[:8, 3, 0:8], in1=bd[:, 1024:1032],
                                                      op=ALU.mult), reads=[psr[3], bd], writes=[o8])
                for c, (c0, cw) in enumerate(((0, 512), (512, 512), (1024, 8))):
                    S.op('pe', lambda e, c=c, c0=c0, cw=cw: e.matmul(ps[:1, 4 + c, 0:cw], lhsT=ones8[:, :],
                                                                     rhs=o8[:, c0:c0 + cw], start=True, stop=True),
                         reads=[ones8, o8], writes=[psr[4 + c]])
                S.op('act', lambda e: e.copy(out=row[:, 0:1024], in_=ps[:1, 4:6, :].rearrange("p b c -> p (b c)")),
                     reads=[psr[4], psr[5]], writes=[row])
                S.op('dve', lambda e: e.reciprocal(out=rrec[:, :], in_=ps[:1, 6, 0:8]), reads=[psr[6]], writes=[rrec])
                S.op('dve', lambda e: e.tensor_tensor(out=orow[:, :].rearrange("p (h d) -> p h d", d=128),
                                                      in0=row[:, 0:1024].rearrange("p (h d) -> p h d", d=128),
                                                      in1=rrec[:, :].unsqueeze(2).broadcast_to([1, 8, 128]), op=ALU.mult),
                     reads=[row, rrec], writes=[orow])
                S.dma('sp', lambda e: e.dma_start(out=self.attn_s_d[4 * s_ + t:4 * s_ + t + 1, :], in_=orow[:, :]),
                      reads=[orow], writes=[self.attn_s_d])
        S.barrier()
        ph.close()

    def phase_convert(self, pairs):
        S = self.S
        ph = contextlib.ExitStack()
        RB = 8
        bufs = [self.sb('cvb%d' % i, [128, RB, D], BF16, ph) for i in range(3)]
        it = 0
        for src, dst in pairs:
            sv = src.t.rearrange("(p r) d -> p r d", p=128)
            dv = dst.t.rearrange("(p r) d -> p r d", p=128)
            for r0 in range(0, 128, RB):
                b_ = bufs[it % 3]
                S.dma('pool', lambda e, b_=b_, sv=sv, r0=r0: e.dma_start(out=b_[:, :, :], in_=sv[:, r0:r0 + RB, :]),
                      reads=[src], writes=[b_])
                S.dma('sp' if it % 2 == 0 else 'act',
                      lambda e, b_=b_, dv=dv, r0=r0: e.dma_start(out=dv[:, r0:r0 + RB, :], in_=b_[:, :, :]),
                      reads=[b_], writes=[dst])
                it += 1
        S.barrier()
        ph.close()

    def finish(self):
        self.S.finish()
        if self.phA is not None:
            self.phA.close()
        self.stack.close()
        return self.nc


def _rope_table():
    half = 16
    inv = (np.float32(500000.0) ** (-np.arange(half, dtype=np.float32) / np.float32(half))).astype(np.float32)
    pos = np.concatenate([np.arange(SEQ), PAST + (np.arange(NS) % 4)]).astype(np.float32)
    ang = (pos[:, None] * inv[None, :]).astype(np.float32)
    c, s_ = np.cos(ang).astype(np.float32), np.sin(ang).astype(np.float32)
    return np.ascontiguousarray(np.concatenate([c, c, -s_, s_], axis=1))


def _mask2():
    kk = np.arange(128)[:, None]
    qq = np.arange(128)[None, :]
    prev = (kk >= qq).astype(np.float32)
    cur = (kk <= qq).astype(np.float32)
    return np.ascontiguousarray(np.concatenate([prev, cur, prev, cur], axis=1))


def _bd():
    m = np.zeros((8, D + 8), np.float32)
    for h in range(8):
        m[h, h * 128:(h + 1) * 128] = 1.0
        m[h, D + h] = 1.0
    return m


def _prep(inp):
    f = lambda a: np.ascontiguousarray(np.asarray(a, dtype=np.float32))
    shared = {
        'rt': _rope_table(), 'identf': np.eye(128, dtype=np.float32),
        'maskd_f': np.triu(np.ones((128, 128), np.float32)),
        'mask4_f': np.ascontiguousarray(np.tile((np.arange(4)[:, None] <= np.arange(4)[None, :]).astype(np.float32), (1, 16))),
        'cache_mla': f(inp['cache_mla'][0].reshape(5120, 128 * 288)), 'a_w_o': f(inp['a_w_o'][0]),
        'iota_d': np.arange(16, dtype=np.float32)[None, :],
        'mask2_f': _mask2(), 'bd_f': _bd(),
        'kv_w': f(inp['kv_w']), 'kv_g_k': f(inp['kv_g_k']), 'b_w_q': f(inp['b_w_q'][0]), 'b_g_q': f(inp['b_g_q'][0]),
        'b_w_o': f(inp['b_w_o'][0]),
        'a_mod_w': f(inp['a_mod_w'][0]), 'f_mod_w0': f(inp['f_mod_w'][0]), 'kv_mod_w': f(inp['kv_mod_w']),
        'b_mod_w': f(inp['b_mod_w'][0]), 'f_mod_w1': f(inp['f_mod_w'][1]),
        'a_mod_b': f(inp['a_mod_b'][0:1]), 'f_mod_b0': f(inp['f_mod_b'][0:1]), 'kv_mod_b': f(inp['kv_mod_b'][None]),
        'b_mod_b': f(inp['b_mod_b'][0:1]), 'f_mod_b1': f(inp['f_mod_b'][1:2]),
        'a_w_dq': f(inp['a_w_dq'][0]), 'a_w_uq': f(inp['a_w_uq'][0]), 'a_w_dkv': f(inp['a_w_dkv'][0]),
        'a_w_uk': f(inp['a_w_uk'][0].reshape(256, 1024)), 'a_w_uv': f(inp['a_w_uv'][0].reshape(256, 1024)),
    }
    for l in range(2):
        shared['f_w_q%d' % l] = f(inp['f_w_q'][l])
        shared['f_subT%d' % l] = f(np.asarray(inp['f_subkeys'][l]).transpose(1, 3, 0, 2).reshape(128, 8, 128))
        shared['f_u%d' % l] = f(inp['f_u'][l])
        shared['f_v%d' % l] = f(inp['f_v'][l])
    for k in ('a_g_cq', 'a_g_ckv', 'a_g_qn', 'a_g_qr', 'a_g_kr', 'a_g_kn'):
        shared[k] = f(inp[k][0:1])
    maps = []
    for c in range(NCORES):
        m = dict(shared)
        xs = np.asarray(inp['x_sample'], np.float32)[4 * c:4 * c + 4].reshape(NS, D)
        m['xin'] = np.ascontiguousarray(np.concatenate([np.asarray(inp['x_prompt'], np.float32)[c], xs], axis=0))
        c5 = np.concatenate([np.asarray(inp['c_prompt'], np.float32)[c:c + 1],
                             np.asarray(inp['c_sample'], np.float32)[4 * c:4 * c + 4]], axis=0)
        m['cT'] = np.ascontiguousarray(c5.reshape(5, 8, 128).transpose(2, 1, 0).reshape(128, 40))
        for nm in ('cache_dil0', 'cache_dil1', 'cache_dil2'):
            cd = np.asarray(inp[nm], np.float32)[4 * c:4 * c + 4]
            m[nm] = np.ascontiguousarray(cd.reshape(4, cd.shape[1], 2, D))
        m['ptT'] = np.ascontiguousarray(np.asarray(inp['page_table'], np.int32)[4 * c:4 * c + 4].T)
        maps.append(m)
    return maps


def run(inp, upto='all', debug=False, cores=None):
    b = Builder(upto=upto, debug=debug)
    nc = b.build()
    print('instructions:', b.S.ninstr, {k: v for k, v in b.S.cnt.items()}, b.S.dcnt)
    maps = _prep(inp)
    used = set(b.dram.keys())
    maps = [{k: v for k, v in m.items() if k in used} for m in maps]
    if cores is not None:
        maps = [maps[c] for c in cores]
    res = run_bass_kernel_spmd(nc, maps, core_ids=list(range(len(maps))))
    return res.results


def kernel(**inputs):
    res = run(inputs)
    Ws = (128, 512, 2048)
    y = np.stack([np.asarray(r['y']) for r in res])
    ml = np.stack([np.asarray(r['mla_rows']) for r in res])
    outs = [np.ascontiguousarray(y[:, :SEQ]), np.ascontiguousarray(y[:, SEQ:].reshape(32, 4, D)),
            np.ascontiguousarray(ml[:, :SEQ])[None], np.ascontiguousarray(ml[:, SEQ:].reshape(32, 4, 288))[None]]
    for g in range(3):
        dp = np.stack([np.asarray(r['dil%d_p' % g]) for r in res]).reshape(8, Ws[g], 2, 8, 128)
        ds = np.concatenate([np.asarray(r['dil%d_s' % g]) for r in res], axis=0).reshape(32, Ws[g], 2, 8, 128)
        outs += [dp, ds]
    return tuple(np.asarray(o, dtype=np.float32) for o in outs)
```

```python
import contextlib
import numpy as np
import concourse.bass as bass
import concourse.mybir as mybir
from concourse.bass_utils import run_bass_kernel_spmd

F32 = mybir.dt.float32
BF16 = mybir.dt.bfloat16
I32 = mybir.dt.int32
U32 = mybir.dt.uint32
ALU = mybir.AluOpType
AF = mybir.ActivationFunctionType
AX = mybir.AxisListType

NCORES = 8
D = 1024
SEQ = 2048
NT = 16
NS = 16
NTOK = SEQ + NS
EPS = 1e-6
PAST = 16384
MOD_OFF = {'a': 0, 'f0': 3072, 'kv': 6144, 'b': 8192, 'f1': 11264}
MOD_TOT = 14336
SAME_ENG_WINDOW = 3


class Res:
    __slots__ = ('name', 'w', 'r')

    def __init__(self, name):
        self.name = name
        self.w = None
        self.r = {}


class TT(Res):
    __slots__ = ('t',)

    def __init__(self, name, t):
        super().__init__(name)
        self.t = t

    def __getitem__(self, k):
        return self.t[k]


class Sched:
    def __init__(self, nc, stack, M=16):
        self.nc = nc
        self.eng = {'pe': nc.tensor, 'dve': nc.vector, 'act': nc.scalar, 'pool': nc.gpsimd, 'sp': nc.sync}
        self.sem = {e: stack.enter_context(nc.semaphore('s_' + e)) for e in self.eng}
        self.cnt = {e: 0 for e in self.eng}
        self.seen = {e: {} for e in self.eng}
        self.M = M
        self.dsem = {q: [stack.enter_context(nc.semaphore('d_%s%d' % (q, i))) for i in range(M)]
                     for q in ('sp', 'act', 'pool')}
        self.dcnt = {q: 0 for q in self.dsem}
        self.duse = {q: [0] * M for q in self.dsem}
        self.ninstr = 0

    def _wait(self, e, tok):
        key, h, v, owner = tok
        if owner == e:
            if e == 'pe' or v <= self.cnt[e] - SAME_ENG_WINDOW:
                return
        if self.seen[e].get(key, 0) >= v:
            return
        self.eng[e].wait_ge(h, v)
        self.seen[e][key] = v
        self.ninstr += 1

    def _deps(self, e, reads, writes):
        for r in reads:
            if r.w is not None:
                self._wait(e, r.w)
        for w in writes:
            if w.w is not None:
                self._wait(e, w.w)
            for tok in w.r.values():
                self._wait(e, tok)

    def _commit(self, tok, reads, writes):
        for r in reads:
            r.r[tok[0]] = tok
        for w in writes:
            w.w = tok
            w.r = {}

    def op(self, e, fn, reads=(), writes=()):
        self._deps(e, reads, writes)
        ins = fn(self.eng[e])
        self.cnt[e] += 1
        ins.then_inc(self.sem[e], 1)
        self.ninstr += 1
        self._commit((e, self.sem[e], self.cnt[e], e), reads, writes)

    def dma(self, q, fn, reads=(), writes=()):
        slot = self.dcnt[q] % self.M
        self.dcnt[q] += 1
        h = self.dsem[q][slot]
        if self.duse[q][slot] > 0:
            self._wait(q, ((q, slot), h, 16 * self.duse[q][slot], None))
        self._deps(q, reads, writes)
        ins = fn(self.eng[q])
        self.duse[q][slot] += 1
        ins.then_inc(h, 16)
        self.ninstr += 1
        self._commit(((q, slot), h, 16 * self.duse[q][slot], None), reads, writes)

    def barrier(self):
        for e in self.eng:
            for q in self.dsem:
                for slot in range(self.M):
                    if self.duse[q][slot] > 0:
                        self._wait(e, ((q, slot), self.dsem[q][slot], 16 * self.duse[q][slot], None))
            for e2 in self.eng:
                if e2 != e and self.cnt[e2] > 0:
                    self._wait(e, (e2, self.sem[e2], self.cnt[e2], e2))

    def finish(self):
        e = 'sp'
        for q in self.dsem:
            for slot in range(self.M):
                if self.duse[q][slot] > 0:
                    self._wait(e, ((q, slot), self.dsem[q][slot], 16 * self.duse[q][slot], None))
        for e2 in self.eng:
            if e2 != e and self.cnt[e2] > 0:
                self._wait(e, (e2, self.sem[e2], self.cnt[e2], e2))


def gk_g(gk, g):
    class _V:
        pass
    v = TTView(gk, gk.t[:, g, :])
    return v


class TTView:
    def __init__(self, parent, ap):
        self.parent, self.ap = parent, ap

    def __getitem__(self, k):
        return self.ap[k]

    @property
    def w(self):
        return self.parent.w

    @w.setter
    def w(self, v):
        self.parent.w = v

    @property
    def r(self):
        return self.parent.r

    @r.setter
    def r(self, v):
        self.parent.r = v


class Builder:
    def __init__(self, upto='all', debug=False):
        self.upto = upto
        self.debug = debug
        self.nc = bass.Bass("TRN2", target_bir_lowering=False)
        self.stack = contextlib.ExitStack()
        self.S = None
        self.dram = {}

    def din(self, name, shape, dt=F32):
        t = self.nc.dram_tensor(name, list(shape), dt, kind="ExternalInput")
        r = TT(name, t.ap())
        self.dram[name] = r
        return r

    def dout(self, name, shape, dt=F32):
        t = self.nc.dram_tensor(name, list(shape), dt, kind="ExternalOutput")
        r = TT(name, t.ap())
        self.dram[name] = r
        return r

    def dscr(self, name, shape, dt=F32):
        kind = "ExternalOutput" if self.debug else "Internal"
        t = self.nc.dram_tensor(name, list(shape), dt, kind=kind)
        r = TT(name, t.ap())
        self.dram[name] = r
        return r

    def sb(self, name, shape, dt=F32, cm=None):
        self._uid = getattr(self, '_uid', 0) + 1
        name = '%s_%d' % (name, self._uid)
        t = (cm or self.stack).enter_context(self.nc.sbuf_tensor(name, list(shape), dt))
        return TT(name, t)

    def rstd(self, ssq, out, n, inv_count, scratch_eng='act'):
        S = self.S
        (sq_t, sq_ap), (o_t, o_ap) = ssq, out
        S.op('act', lambda e: e.activation(out=o_ap, in_=sq_ap, func=AF.Sqrt, scale=inv_count,
                                           bias=self.eps_t[:n, 0:1]), reads=[sq_t, self.eps_t], writes=[o_t])
        S.op('dve', lambda e: e.reciprocal(out=o_ap, in_=o_ap), reads=[o_t], writes=[o_t])

    def transposes(self, src, src_aps, n, ps_bank, dst, dst_ap, copy_eng='act'):
        S = self.S
        psb = self.ps[:, ps_bank, :].bitcast(BF16)
        wmax = max(a.shape[-1] for a in src_aps)
        for i, a in enumerate(src_aps):
            w = a.shape[-1]
            S.op('pe', lambda e, a=a, i=i, w=w: e.transpose(out=psb[:w, i * 128:i * 128 + n], in_=a,
                                                             identity=self.ident[:n, :n]),
                 reads=[src, self.ident], writes=[self.psr[ps_bank]])
        pv = psb.rearrange("p (k c) -> p k c", c=128)[:wmax, :len(src_aps), :n]
        if copy_eng == 'act':
            S.op('act', lambda e: e.copy(out=dst_ap, in_=pv), reads=[self.psr[ps_bank]], writes=[dst])
        else:
            S.op('dve', lambda e: e.tensor_copy(out=dst_ap, in_=pv), reads=[self.psr[ps_bank]], writes=[dst])

    def _mk_rope(self, t32, t32b):
        S = self.S

        def rope(src_t, src, dst_t, dst, tb, n, H):
            cc = tb[:n, 0:32].unsqueeze(1).broadcast_to([n, H, 32])
            s_lo = tb[:n, 32:48].unsqueeze(1).broadcast_to([n, H, 16])
            s_hi = tb[:n, 48:64].unsqueeze(1).broadcast_to([n, H, 16])
            S.op('dve', lambda e: e.tensor_tensor(out=t32[:n, :H, :], in0=src, in1=cc, op=ALU.mult),
                 reads=[src_t, tb], writes=[t32])
            S.op('dve', lambda e: e.tensor_tensor(out=t32b[:n, :H, 0:16], in0=src[:, :, 16:32], in1=s_lo, op=ALU.mult),
                 reads=[src_t, tb], writes=[t32b])
            S.op('dve', lambda e: e.tensor_tensor(out=t32b[:n, :H, 16:32], in0=src[:, :, 0:16], in1=s_hi, op=ALU.mult),
                 reads=[src_t, tb], writes=[t32b])
            S.op('dve', lambda e: e.tensor_tensor(out=dst, in0=t32[:n, :H, :], in1=t32b[:n, :H, :], op=ALU.add),
                 reads=[t32, t32b], writes=[dst_t])

        self.rope = rope

    def rt_all_tile(self, ti):
        return TTView(self.rt_all, self.rt_all.t[:, ti, :])

    def build(self):
        nc = self.nc
        st = self.stack
        self.S = S = Sched(nc, st)
        st.enter_context(nc.Block())
        dbg = self.debug

        xin = self.din('xin', [NTOK, D])
        cT = self.din('cT', [128, 40])
        rt = self.din('rt', [NTOK, 64])
        identf = self.din('identf', [128, 128])
        modw = {'a': self.din('a_mod_w', [D, 3072]), 'f0': self.din('f_mod_w0', [D, 3072]),
                'kv': self.din('kv_mod_w', [D, 2048]), 'b': self.din('b_mod_w', [D, 3072]),
                'f1': self.din('f_mod_w1', [D, 3072])}
        modb = {'a': self.din('a_mod_b', [1, 3072]), 'f0': self.din('f_mod_b0', [1, 3072]),
                'kv': self.din('kv_mod_b', [1, 2048]), 'b': self.din('b_mod_b', [1, 3072]),
                'f1': self.din('f_mod_b1', [1, 3072])}
        a_w_dq = self.din('a_w_dq', [D, 384])
        a_w_uq = self.din('a_w_uq', [384, 1536])
        a_w_dkv = self.din('a_w_dkv', [D, 288])
        a_w_uk = self.din('a_w_uk', [256, 1024])
        a_w_uv = self.din('a_w_uv', [256, 1024])
        gains = {k: self.din(k, [1, w]) for k, w in
                 [('a_g_cq', 384), ('a_g_ckv', 256), ('a_g_qn', 64), ('a_g_qr', 32), ('a_g_kr', 32), ('a_g_kn', 64)]}

        y_out = self.dout('y', [NTOK, D])
        mla_out = self.dout('mla_rows', [NTOK, 288])

        modv = self.dscr('modv', [5, MOD_TOT])
        qpad_d = self.dscr('qpad_d', [NTOK, 16 * 128], BF16)
        kpad_d = self.dscr('kpad_d', [NTOK, 16 * 128], BF16)
        vaug_d = self.dscr('vaug_d', [NTOK, 16 * 65], BF16)

        self.ps_t = st.enter_context(nc.psum_tensor('ps', [128, 8, 512], F32))
        self.ps = self.ps_t
        self.psr = [Res('psb%d' % i) for i in range(8)]
        self.ident = self.sb('ident', [128, 128], BF16)
        self.eps_t = self.sb('eps', [128, 1], F32)
        S.dma('pool', lambda e: e.dma_start(out=self.ident[:, :], in_=identf[:, :]), reads=[identf], writes=[self.ident])
        S.op('dve', lambda e: e.memset(self.eps_t[:, :], EPS), writes=[self.eps_t])

        with contextlib.ExitStack() as ph:
            cTs = self.sb('cTs', [128, 40], F32, ph)
            silT = self.sb('silT', [128, 40], F32, ph)
            wb = [self.sb('mw%d' % i, [128, 8, 512], F32, ph) for i in range(2)]
            bb = [self.sb('mb%d' % i, [5, 512], F32, ph) for i in range(2)]
            ob = [self.sb('mo%d' % i, [5, 512], F32, ph) for i in range(2)]
            S.dma('sp', lambda e: e.dma_start(out=cTs[:, :], in_=cT[:, :]), reads=[cT], writes=[cTs])
            S.op('act', lambda e: e.activation(out=silT[:, :], in_=cTs[:, :], func=AF.Silu), reads=[cTs], writes=[silT])
            it = 0
            for key in ('a', 'f0', 'kv', 'b', 'f1'):
                W, Bv = modw[key], modb[key]
                N = W.t.shape[1]
                for c in range(N // 512):
                    w_, b_, o_ = wb[it % 2], bb[it % 2], ob[it % 2]
                    pb = it % 2
                    n0 = c * 512
                    S.dma('sp', lambda e, w_=w_, W=W, n0=n0: e.dma_start(
                        out=w_[:, :, :], in_=W[:, n0:n0 + 512].rearrange("(k p) n -> p k n", p=128)),
                        reads=[W], writes=[w_])
                    S.dma('act', lambda e, b_=b_, Bv=Bv, n0=n0: e.dma_start(
                        out=b_[:, :], in_=Bv[0:1, n0:n0 + 512].broadcast_to([5, 512])), reads=[Bv], writes=[b_])
                    for k in range(8):
                        S.op('pe', lambda e, k=k, w_=w_, pb=pb: e.matmul(
                            self.ps[:5, pb, :], lhsT=silT[:, k * 5:(k + 1) * 5], rhs=w_[:, k, :],
                            start=(k == 0), stop=(k == 7)), reads=[silT, w_], writes=[self.psr[pb]])
                    S.op('dve', lambda e, o_=o_, b_=b_, pb=pb: e.tensor_tensor(
                        out=o_[:, :], in0=self.ps[:5, pb, :], in1=b_[:, :], op=ALU.add),
                        reads=[self.psr[pb], b_], writes=[o_])
                    off = MOD_OFF[key] + n0
                    S.dma('sp', lambda e, o_=o_, off=off: e.dma_start(out=modv[:, off:off + 512], in_=o_[:, :]),
                          reads=[o_], writes=[modv])
                    it += 1
            S.barrier()
        if self.upto == 'M':
            return self.finish()

        self.pm = self.sb('pm', [128, 3, D], F32)
        self.sm = self.sb('sm', [128, 3, D], F32)

        def load_mod(key, nparts):
            off = MOD_OFF[key]
            for j in range(nparts):
                S.dma('sp', lambda e, j=j: e.dma_start(
                    out=self.pm[:, j, :], in_=modv[0:1, off + j * D: off + (j + 1) * D].broadcast_to([128, D])),
                    reads=[modv], writes=[self.pm])
                for s in range(4):
                    S.dma('act', lambda e, j=j, s=s: e.dma_start(
                        out=self.sm[4 * s:4 * s + 4, j, :],
                        in_=modv[1 + s:2 + s, off + j * D: off + (j + 1) * D].broadcast_to([4, D])),
                        reads=[modv], writes=[self.sm])
            S.op('dve', lambda e: e.tensor_scalar_add(out=self.pm[:, 1, :], in0=self.pm[:, 1, :], scalar1=1.0),
                 reads=[self.pm], writes=[self.pm])
            S.op('dve', lambda e: e.tensor_scalar_add(out=self.sm[:NS, 1, :], in0=self.sm[:NS, 1, :], scalar1=1.0),
                 reads=[self.sm], writes=[self.sm])

        self.load_mod = load_mod
        self.xb = [self.sb('xb%d' % i, [128, D], F32) for i in range(2)]
        self.junk = self.sb('junk', [128, 1536], F32)
        self.tmp = self.sb('tmpf', [128, D], F32)
        self.hb = self.sb('hb', [128, D], BF16)
        self.hT = self.sb('hT', [128, 8, 128], BF16)
        self.st1 = self.sb('st1', [128, 8], F32)

        def ada_tile(ti, n, row0):
            x = self.xb[ti % 2]
            md = self.pm if n == 128 else self.sm
            S.dma('sp', lambda e: e.dma_start(out=x[:n, :], in_=self.xsrc[row0:row0 + n, :]), reads=[self.xsrc],
                  writes=[x])
            S.op('act', lambda e: e.activation(out=self.junk[:n, :D], in_=x[:n, :], func=AF.Square,
                                               accum_out=self.st1[:n, 0:1]), reads=[x], writes=[self.junk, self.st1])
            self.rstd((self.st1, self.st1[:n, 0:1]), (self.st1, self.st1[:n, 1:2]), n, 1.0 / D)
            S.op('dve', lambda e: e.scalar_tensor_tensor(out=self.tmp[:n, :], in0=x[:n, :], scalar=self.st1[:n, 1:2],
                                                         in1=md[:n, 1, :], op0=ALU.mult, op1=ALU.mult),
                 reads=[x, self.st1, md], writes=[self.tmp])
            S.op('dve', lambda e: e.tensor_tensor(out=self.hb[:n, :], in0=self.tmp[:n, :], in1=md[:n, 0, :],
                                                  op=ALU.add), reads=[self.tmp, md], writes=[self.hb])
            self.transposes(self.hb, [self.hb[:n, k * 128:(k + 1) * 128] for k in range(8)], n, 0, self.hT,
                            self.hT[:, :, :n])
            return x

        self.ada_tile = ada_tile
        self.xsrc = xin
        self.attn = self.sb('attn', [128, NT + 1, D], BF16)
        self.rt_all = self.sb('rt_all', [128, NT + 1, 64], F32)
        S.dma('sp', lambda e: e.dma_start(out=self.rt_all[:, 0:NT, :], in_=rt[0:SEQ, :].rearrange("(t p) c -> p t c", p=128)),
              reads=[rt], writes=[self.rt_all])
        S.dma('sp', lambda e: e.dma_start(out=self.rt_all[:NS, NT, :], in_=rt[SEQ:NTOK, :]), reads=[rt], writes=[self.rt_all])
        self.phA = None
        self.phase_A1(a_w_dq, a_w_uq, a_w_dkv, a_w_uk, a_w_uv, gains, rt, mla_out, qpad_d, kpad_d, vaug_d)
        if self.upto == 'A1':
            return self.finish()
        self.attn_s_d = self.dscr('attn_s_d', [NS, D], BF16)
        maskd_f = self.din('maskd_f', [128, 128])
        self.iota_d = self.din('iota_d', [1, 16])
        mask4_f = self.din('mask4_f', [4, 64])
        cache2d = self.din('cache_mla', [5120, 128 * 288])
        ptT = self.din('ptT', [128, 4], I32)
        a_w_o = self.din('a_w_o', [D, D])
        x1 = self.dscr('x1', [NTOK, D])
        self.phase_A2(qpad_d, kpad_d, vaug_d, maskd_f)
        if self.upto == 'A2':
            self.dbg_attn = self.dscr('dbg_attn', [NTOK, D], BF16)
            for ti in range(NT):
                S.dma('sp', lambda e, ti=ti: e.dma_start(out=self.dbg_attn[ti * 128:(ti + 1) * 128, :],
                                                         in_=self.attn[:, ti, :]), reads=[self.attn],
                      writes=[self.dbg_attn])
            return self.finish()
        self.phase_A3(cache2d, ptT, qpad_d, kpad_d, vaug_d, mask4_f)
        self.phA.close()
        self.phase_oproj(a_w_o, x1)
        if self.upto == 'A':
            return self.finish()
        f_w_q = [self.din('f_w_q%d' % l, [D, D]) for l in range(2)]
        f_subT = [self.din('f_subT%d' % l, [128, 8, 128]) for l in range(2)]
        f_u = [self.din('f_u%d' % l, [16384, D]) for l in range(2)]
        f_v = [self.din('f_v%d' % l, [16384, D]) for l in range(2)]
        x2 = self.dscr('x2', [NTOK, D])
        uv_d = [self.dscr('uv_d%d' % l, [16384, 2, D], BF16) for l in range(2)]
        self.phase_convert([(f_u[0], uv_d[0], 0), (f_v[0], uv_d[0], 1), (f_u[1], uv_d[1], 0), (f_v[1], uv_d[1], 1)])
        f_u, f_v = uv_d, uv_d
        self.xsrc = x1
        self.phase_peer('f0', f_w_q[0], f_subT[0], f_u[0], f_v[0], x2)
        if self.upto == 'F0':
            return self.finish()
        kv_w = self.din('kv_w', [D, 6144])
        kv_g_k = self.din('kv_g_k', [3, 128])
        b_w_q = self.din('b_w_q', [D, 3072])
        b_g_q = self.din('b_g_q', [3, 128])
        b_w_o = self.din('b_w_o', [D, D])
        mask2_f = self.din('mask2_f', [128, 512])
        bd_f = self.din('bd_f', [8, D + 8])
        Ws = (128, 512, 2048)
        cache_dil = [self.din('cache_dil%d' % g, [4, Ws[g], 2, D]) for g in range(3)]
        dil_p = [self.dout('dil%d_p' % g, [Ws[g], 2, D]) for g in range(3)]
        dil_s = [self.dout('dil%d_s' % g, [4, Ws[g], 2, D]) for g in range(3)]
        kd = self.dscr('kd', [3, NTOK, D], BF16)
        vd = self.dscr('vd', [3, NTOK, D], BF16)
        qd = self.dscr('qd', [3, NTOK, D], BF16)
        dacc = self.dscr('dacc', [3, SEQ, 8 * 129])
        x3 = self.dscr('x3', [NTOK, D])
        for g in range(3):
            W = Ws[g]
            for s_ in range(4):
                for r0 in range(0, W - 4, 256):
                    nr = min(256, W - 4 - r0)
                    S.dma('act', lambda e, g=g, s_=s_, r0=r0, nr=nr: e.dma_start(
                        out=dil_s[g][s_, r0:r0 + nr, :, :].rearrange("r t d -> r (t d)"),
                        in_=cache_dil[g][s_, 4 + r0:4 + r0 + nr, :, :].rearrange("r t d -> r (t d)")),
                        reads=[cache_dil[g]], writes=[dil_s[g]])
        self.xsrc = x2
        self.phase_KV(kv_w, kv_g_k, rt, dil_p, dil_s, kd, vd)
        if self.upto == 'KV':
            return self.finish()
        self.phase_B1(b_w_q, b_g_q, qd)
        self.phase_B2(qd, kd, vd, dacc, mask2_f)
        self.phase_B3(qd, cache_dil, dil_s, bd_f)
        self.phase_oproj(b_w_o, x3)
        if self.upto == 'B':
            return self.finish()
        self.xsrc = x3
        self.phase_peer('f1', f_w_q[1], f_subT[1], f_u[1], f_v[1], y_out)
        return self.finish()

    def phase_A1(self, a_w_dq, a_w_uq, a_w_dkv, a_w_uk, a_w_uv, gains, rt, mla_out, qpad_d, kpad_d, vaug_d):
        S = self.S
        ph1 = contextlib.ExitStack()
        self.phA = ph = contextlib.ExitStack()
        wuk = self.sb('wuk', [128, 2, 1024], BF16, ph)
        wuv = self.sb('wuv', [128, 2, 1024], BF16, ph)
        gt = {}
        for k, g in gains.items():
            w = g.t.shape[1]
            gt[k] = self.sb('t_' + k, [128, w], F32, ph)
            S.dma('act', lambda e, k=k, g=g, w=w: e.dma_start(out=gt[k][:, :], in_=g[0:1, :].broadcast_to([128, w])),
                  reads=[g], writes=[gt[k]])
        qf = self.sb('qf', [128, 1536], F32, ph)
        st = self.sb('stA', [128, 96], F32, ph)
        qpad = self.sb('qpad', [128, 16, 128], BF16, ph)
        kpad = self.sb('kpad', [128, 16, 128], BF16, ph)
        vaug = self.sb('vaug', [128, 16, 65], BF16, ph)
        lat = self.sb('lat', [128, 288], F32, ph)
        latb = self.sb('latb', [128, 288], BF16, ph)
        ckvT = self.sb('ckvT', [128, 2, 128], BF16, ph)
        t32 = self.sb('t32', [128, 16, 32], F32, ph)
        t32b = self.sb('t32b', [128, 16, 32], F32, ph)
        wdq = self.sb('wdq', [128, 8, 384], BF16, ph1)
        wuq = self.sb('wuq', [128, 3, 1536], BF16, ph1)
        wdkv = self.sb('wdkv', [128, 8, 288], BF16, ph1)
        for w_, W in ((wdq, a_w_dq), (wuq, a_w_uq), (wdkv, a_w_dkv), (wuk, a_w_uk), (wuv, a_w_uv)):
            S.dma('pool', lambda e, w_=w_, W=W: e.dma_start(
                out=w_[:, :, :], in_=W[:, :].rearrange("(k p) n -> p k n", p=128)), reads=[W], writes=[w_])
        rtb = [self.sb('rtb%d' % i, [128, 64], F32, ph1) for i in range(2)]
        cqb = self.sb('cqb', [128, 384], BF16, ph1)
        cqT = self.sb('cqT', [128, 3, 128], BF16, ph1)
        S.op('pool', lambda e: e.memset(qpad[:, :, :], 0.0), writes=[qpad])
        S.op('pool', lambda e: e.memset(kpad[:, :, :], 0.0), writes=[kpad])
        S.op('pool', lambda e: e.memset(vaug[:, :, :], 1.0), writes=[vaug])
        self.load_mod('a', 3)
        ps, psr = self.ps, self.psr

        def rope(src_t, src, dst_t, dst, tb, n, H):
            cc = tb[:n, 0:32].unsqueeze(1).broadcast_to([n, H, 32])
            s_lo = tb[:n, 32:48].unsqueeze(1).broadcast_to([n, H, 16])
            s_hi = tb[:n, 48:64].unsqueeze(1).broadcast_to([n, H, 16])
            S.op('dve', lambda e: e.tensor_tensor(out=t32[:n, :H, :], in0=src, in1=cc, op=ALU.mult),
                 reads=[src_t, tb], writes=[t32])
            S.op('dve', lambda e: e.tensor_tensor(out=t32b[:n, :H, 0:16], in0=src[:, :, 16:32], in1=s_lo, op=ALU.mult),
                 reads=[src_t, tb], writes=[t32b])
            S.op('dve', lambda e: e.tensor_tensor(out=t32b[:n, :H, 16:32], in0=src[:, :, 0:16], in1=s_hi, op=ALU.mult),
                 reads=[src_t, tb], writes=[t32b])
            S.op('dve', lambda e: e.tensor_tensor(out=dst, in0=t32[:n, :H, :], in1=t32b[:n, :H, :], op=ALU.add),
                 reads=[t32, t32b], writes=[dst_t])

        self.rope = rope

        def expand_tile(n, src_t, src):
            S.op('act', lambda e: e.copy(out=latb[:n, :], in_=src), reads=[src_t], writes=[latb])
            self.transposes(latb, [latb[:n, k * 128:(k + 1) * 128] for k in range(2)], n, 0, ckvT, ckvT[:, :, :n])
            for c in range(2):
                for k in range(2):
                    S.op('pe', lambda e, c=c, k=k: e.matmul(ps[:n, 6 + c, :], lhsT=ckvT[:, k, :n],
                                                            rhs=wuk[:, k, c * 512:(c + 1) * 512],
                                                            start=(k == 0), stop=(k == 1)),
                         reads=[ckvT, wuk], writes=[psr[6 + c]])
            for c in range(2):
                for k in range(2):
                    S.op('pe', lambda e, c=c, k=k: e.matmul(ps[:n, 1 + c, :], lhsT=ckvT[:, k, :n],
                                                            rhs=wuv[:, k, c * 512:(c + 1) * 512],
                                                            start=(k == 0), stop=(k == 1)),
                         reads=[ckvT, wuv], writes=[psr[1 + c]])
            kraw = ps[:n, 6:8, :].rearrange("p b c -> p (b c)")
            k3 = kraw.rearrange("p (h d) -> p h d", d=64)
            S.op('act', lambda e: e.activation(out=self.junk[:n, 0:1024], in_=kraw, func=AF.Square),
                 reads=[psr[6], psr[7]], writes=[self.junk])
            S.op('dve', lambda e: e.tensor_reduce(out=st[:n, 70:86],
                                                  in_=self.junk[:n, 0:1024].rearrange("p (h d) -> p h d", d=64),
                                                  axis=AX.X, op=ALU.add), reads=[self.junk], writes=[st])
            self.rstd((st, st[:n, 70:86]), (st, st[:n, 70:86]), n, 1.0 / 64)
            qk3 = qf[:n, 0:1024].rearrange("p (h d) -> p h d", d=64)
            S.op('dve', lambda e: e.tensor_tensor(out=qk3, in0=k3, in1=st[:n, 70:86].unsqueeze(2).broadcast_to([n, 16, 64]),
                                                  op=ALU.mult), reads=[psr[6], psr[7], st], writes=[qf])
            S.op('dve', lambda e: e.tensor_tensor(out=kpad[:n, :, 0:64], in0=qk3,
                                                  in1=gt['a_g_kn'][:n, :].unsqueeze(1).broadcast_to([n, 16, 64]),
                                                  op=ALU.mult), reads=[qf, gt['a_g_kn']], writes=[kpad])
            S.op('dve', lambda e: e.tensor_copy(out=kpad[:n, :, 64:96],
                                                in_=src[:, 256:288].unsqueeze(1).broadcast_to([n, 16, 32])),
                 reads=[src_t], writes=[kpad])
            S.op('act', lambda e: e.copy(out=vaug[:n, :, 0:64],
                                         in_=ps[:n, 1:3, :].rearrange("p b (h d) -> p (b h) d", d=64)),
                 reads=[psr[1], psr[2]], writes=[vaug])

        self.expand_tile = expand_tile
        self.kpad, self.vaug = kpad, vaug
        for ti in range(NT + 1):
            n = 128 if ti < NT else NS
            row0 = ti * 128
            tb = rtb[ti % 2]
            S.dma('act', lambda e: e.dma_start(out=tb[:n, :], in_=rt[row0:row0 + n, :]), reads=[rt], writes=[tb])
            self.ada_tile(ti, n, row0)
            hT = self.hT
            for k in range(8):
                S.op('pe', lambda e, k=k: e.matmul(ps[:n, 1, 0:384], lhsT=hT[:, k, :n], rhs=wdq[:, k, :],
                                                  start=(k == 0), stop=(k == 7)), reads=[hT, wdq], writes=[psr[1]])
            for k in range(8):
                S.op('pe', lambda e, k=k: e.matmul(ps[:n, 2, 0:288], lhsT=hT[:, k, :n], rhs=wdkv[:, k, :],
                                                  start=(k == 0), stop=(k == 7)), reads=[hT, wdkv], writes=[psr[2]])
            S.op('act', lambda e: e.activation(out=self.junk[:n, 0:384], in_=ps[:n, 1, 0:384], func=AF.Square,
                                               accum_out=st[:n, 0:1]), reads=[psr[1]], writes=[self.junk, st])
            self.rstd((st, st[:n, 0:1]), (st, st[:n, 1:2]), n, 1.0 / 384)
            S.op('dve', lambda e: e.scalar_tensor_tensor(out=cqb[:n, :], in0=ps[:n, 1, 0:384], scalar=st[:n, 1:2],
                                                         in1=gt['a_g_cq'][:n, :], op0=ALU.mult, op1=ALU.mult),
                 reads=[psr[1], st, gt['a_g_cq']], writes=[cqb])
            self.transposes(cqb, [cqb[:n, k * 128:(k + 1) * 128] for k in range(3)], n, 0, cqT, cqT[:, :, :n])
            for c in range(3):
                for k in range(3):
                    S.op('pe', lambda e, c=c, k=k: e.matmul(ps[:n, 3 + c, :], lhsT=cqT[:, k, :n],
                                                            rhs=wuq[:, k, c * 512:(c + 1) * 512],
                                                            start=(k == 0), stop=(k == 2)),
                         reads=[cqT, wuq], writes=[psr[3 + c]])
            qraw = ps[:n, 3:6, :].rearrange("p b c -> p (b c)")
            q3 = qraw.rearrange("p (h d) -> p h d", d=96)
            S.op('act', lambda e: e.activation(out=self.junk[:n, :], in_=qraw, func=AF.Square),
                 reads=[psr[3], psr[4], psr[5]], writes=[self.junk])
            j3 = self.junk[:n, :].rearrange("p (h d) -> p h d", d=96)
            S.op('dve', lambda e: e.tensor_reduce(out=st[:n, 2:18], in_=j3[:, :, 0:64], axis=AX.X, op=ALU.add),
                 reads=[self.junk], writes=[st])
            S.op('dve', lambda e: e.tensor_reduce(out=st[:n, 18:34], in_=j3[:, :, 64:96], axis=AX.X, op=ALU.add),
                 reads=[self.junk], writes=[st])
            self.rstd((st, st[:n, 2:18]), (st, st[:n, 34:50]), n, 1.0 / 64)
            self.rstd((st, st[:n, 18:34]), (st, st[:n, 50:66]), n, 1.0 / 32)
            qf3 = qf[:n, :].rearrange("p (h d) -> p h d", d=96)
            S.op('dve', lambda e: e.tensor_tensor(out=qf3[:, :, 0:64], in0=q3[:, :, 0:64],
                                                  in1=st[:n, 34:50].unsqueeze(2).broadcast_to([n, 16, 64]),
                                                  op=ALU.mult), reads=[psr[3], psr[4], psr[5], st], writes=[qf])
            S.op('dve', lambda e: e.tensor_tensor(out=qpad[:n, :, 0:64], in0=qf3[:, :, 0:64],
                                                  in1=gt['a_g_qn'][:n, :].unsqueeze(1).broadcast_to([n, 16, 64]),
                                                  op=ALU.mult), reads=[qf, gt['a_g_qn']], writes=[qpad])
            S.op('dve', lambda e: e.tensor_tensor(out=qf3[:, :, 64:96], in0=q3[:, :, 64:96],
                                                  in1=st[:n, 50:66].unsqueeze(2).broadcast_to([n, 16, 32]),
                                                  op=ALU.mult), reads=[psr[3], psr[4], psr[5], st], writes=[qf])
            S.op('dve', lambda e: e.tensor_tensor(out=qf3[:, :, 64:96], in0=qf3[:, :, 64:96],
                                                  in1=gt['a_g_qr'][:n, :].unsqueeze(1).broadcast_to([n, 16, 32]),
                                                  op=ALU.mult), reads=[qf, gt['a_g_qr']], writes=[qf])
            rope(qf, qf3[:, :, 64:96], qpad, qpad[:n, :, 64:96], tb, n, 16)
            S.dma('sp', lambda e: e.dma_start(out=qpad_d[row0:row0 + n, :],
                                              in_=qpad[:n, :, :].rearrange("p h d -> p (h d)")),
                  reads=[qpad], writes=[qpad_d])
            S.op('act', lambda e: e.activation(out=self.junk[:n, 0:256], in_=ps[:n, 2, 0:256], func=AF.Square,
                                               accum_out=st[:n, 66:67]), reads=[psr[2]], writes=[self.junk, st])
            S.op('act', lambda e: e.activation(out=self.junk[:n, 256:288], in_=ps[:n, 2, 256:288], func=AF.Square,
                                               accum_out=st[:n, 67:68]), reads=[psr[2]], writes=[self.junk, st])
            self.rstd((st, st[:n, 66:67]), (st, st[:n, 68:69]), n, 1.0 / 256)
            self.rstd((st, st[:n, 67:68]), (st, st[:n, 69:70]), n, 1.0 / 32)
            S.op('dve', lambda e: e.scalar_tensor_tensor(out=lat[:n, 0:256], in0=ps[:n, 2, 0:256], scalar=st[:n, 68:69],
                                                         in1=gt['a_g_ckv'][:n, :], op0=ALU.mult, op1=ALU.mult),
                 reads=[psr[2], st, gt['a_g_ckv']], writes=[lat])
            S.op('dve', lambda e: e.scalar_tensor_tensor(out=qf[:n, 0:32], in0=ps[:n, 2, 256:288], scalar=st[:n, 69:70],
                                                         in1=gt['a_g_kr'][:n, :], op0=ALU.mult, op1=ALU.mult),
                 reads=[psr[2], st, gt['a_g_kr']], writes=[qf])
            rope(qf, qf[:n, 0:32].unsqueeze(1), lat, lat[:n, 256:288].unsqueeze(1), tb, n, 1)
            S.dma('sp', lambda e: e.dma_start(out=mla_out[row0:row0 + n, :], in_=lat[:n, :]), reads=[lat],
                  writes=[mla_out])
            expand_tile(n, lat, lat[:n, :])
            S.dma('sp', lambda e: e.dma_start(out=kpad_d[row0:row0 + n, :],
                                              in_=kpad[:n, :, :].rearrange("p h d -> p (h d)")),
                  reads=[kpad], writes=[kpad_d])
            S.dma('sp', lambda e: e.dma_start(out=vaug_d[row0:row0 + n, :],
                                              in_=vaug[:n, :, :].rearrange("p h d -> p (h d)")),
                  reads=[vaug], writes=[vaug_d])
        S.barrier()
        ph1.close()

    def phase_A2(self, qpad_d, kpad_d, vaug_d, maskd_f):
        S = self.S
        ps, psr = self.ps, self.psr
        ph = contextlib.ExitStack()
        qh = self.sb('qh', [128, 16, 128], BF16, ph)
        kh = self.sb('kh', [128, 16, 128], BF16, ph)
        vh = self.sb('vh', [128, 16, 65], BF16, ph)
        QT = self.sb('QT', [128, 2048], BF16, ph)
        KT = self.sb('KT', [128, 2048], BF16, ph)
        PT = [self.sb('PT%d' % i, [128, 512], BF16, ph) for i in range(4)]
        rec = self.sb('recA2', [128, 2], F32, ph)
        maskd = self.sb('maskd', [128, 128], BF16, ph)
        S.dma('pool', lambda e: e.dma_start(out=maskd[:, :], in_=maskd_f[:, :]), reads=[maskd_f], writes=[maskd])
        scale = 96.0 ** -0.5
        sbanks = [2, 3, 6, 7]
        cnt = 0
        for h in range(16):
            S.dma('sp', lambda e: e.dma_start(
                out=qh[:, :, :], in_=qpad_d[0:SEQ, h * 128:(h + 1) * 128].rearrange("(t p) d -> p t d", p=128)),
                reads=[qpad_d], writes=[qh])
            S.dma('act', lambda e: e.dma_start(
                out=kh[:, :, :], in_=kpad_d[0:SEQ, h * 128:(h + 1) * 128].rearrange("(t p) d -> p t d", p=128)),
                reads=[kpad_d], writes=[kh])
            S.dma('sp', lambda e: e.dma_start(
                out=vh[:, :, :], in_=vaug_d[0:SEQ, h * 65:(h + 1) * 65].rearrange("(t p) d -> p t d", p=128)),
                reads=[vaug_d], writes=[vh])
            for g in range(2):
                self.transposes(qh, [qh[:, 8 * g + j, :] for j in range(8)], 128, g, QT,
                                QT[:, g * 1024:(g + 1) * 1024].rearrange("p (k c) -> p k c", c=128), copy_eng='dve')
            for g in range(2):
                self.transposes(kh, [kh[:, 8 * g + j, :] for j in range(8)], 128, g, KT,
                                KT[:, g * 1024:(g + 1) * 1024].rearrange("p (k c) -> p k c", c=128), copy_eng='act')
            for i in range(16):
                ob = 4 + (i % 2)
                for j0 in range(0, i + 1, 4):
                    jn = min(4, i + 1 - j0)
                    sbk = sbanks[cnt % 4]
                    pt = PT[cnt % 4]
                    cnt += 1
                    for jj in range(jn):
                        j = j0 + jj
                        S.op('pe', lambda e, jj=jj, j=j, sbk=sbk: e.matmul(
                            ps[:, sbk, jj * 128:(jj + 1) * 128], lhsT=KT[:, j * 128:(j + 1) * 128],
                            rhs=QT[:, i * 128:(i + 1) * 128], start=True, stop=True),
                            reads=[KT, QT], writes=[psr[sbk]])
                    S.op('act', lambda e, sbk=sbk, pt=pt, jn=jn: e.activation(
                        out=pt[:, 0:jn * 128], in_=ps[:, sbk, 0:jn * 128], func=AF.Exp, scale=scale),
                        reads=[psr[sbk]], writes=[pt])
                    if j0 + jn - 1 == i:
                        S.op('dve', lambda e, pt=pt, jn=jn: e.tensor_tensor(
                            out=pt[:, (jn - 1) * 128:jn * 128], in0=pt[:, (jn - 1) * 128:jn * 128], in1=maskd[:, :],
                            op=ALU.mult), reads=[pt, maskd], writes=[pt])
                    for jj in range(jn):
                        j = j0 + jj
                        S.op('pe', lambda e, jj=jj, j=j, pt=pt: e.matmul(
                            ps[:, ob, 0:65], lhsT=pt[:, jj * 128:(jj + 1) * 128], rhs=vh[:, j, :],
                            start=(j == 0), stop=(j == i)), reads=[pt, vh], writes=[psr[ob]])
                S.op('dve', lambda e: e.reciprocal(out=rec[:, 0:1], in_=ps[:, ob, 64:65]), reads=[psr[ob]], writes=[rec])
                S.op('dve', lambda e: e.tensor_scalar(out=self.attn[:, i, h * 64:(h + 1) * 64], in0=ps[:, ob, 0:64],
                                                      scalar1=rec[:, 0:1], scalar2=None, op0=ALU.mult),
                     reads=[psr[ob], rec], writes=[self.attn])
        S.barrier()
        ph.close()

    def phase_A3(self, cache2d, ptT, qpad_d, kpad_d, vaug_d, mask4_f):
        S = self.S
        ps, psr = self.ps, self.psr
        ph = contextlib.ExitStack()
        R = 8
        idx = self.sb('pidx', [128, 4], I32, ph)
        pgb = [self.sb('pg%d' % i, [128, R, 288], F32, ph) for i in range(2)]
        kT = self.sb('kTs', [128, 16, 128], BF16, ph)
        QTs = self.sb('QTs', [128, 16, 4], BF16, ph)
        q4 = self.sb('q4', [4, 16, 128], BF16, ph)
        k4 = self.sb('k4', [4, 16, 128], BF16, ph)
        v4 = self.sb('v4', [4, 16, 65], BF16, ph)
        PTs = self.sb('PTs', [128, 64], BF16, ph)
        acc = self.sb('acc', [4, 3, 512], F32, ph)
        mask4 = self.sb('mask4', [4, 64], BF16, ph)
        rec = self.sb('recA3', [4, 16], F32, ph)
        o4 = self.sb('o4', [4, 1024], BF16, ph)
        scale = 96.0 ** -0.5
        S.dma('sp', lambda e: e.dma_start(out=idx[:, :], in_=ptT[:, :]), reads=[ptT], writes=[idx])
        S.dma('pool', lambda e: e.dma_start(out=mask4[:, :], in_=mask4_f[:, :]), reads=[mask4_f], writes=[mask4])
        NBLK = 128 // R
        idxf = self.sb('pidxf', [128, 4], F32, ph)
        io16 = self.sb('io16a', [128, NBLK], F32, ph)
        idxaf = self.sb('pidxaf', [128, 4, NBLK], F32, ph)
        idxall = self.sb('pidxall', [128, 4, NBLK], I32, ph)
        S.dma('sp', lambda e: e.dma_start(out=io16[:, :], in_=self.iota_d[0:1, 0:NBLK].broadcast_to([128, NBLK])),
              reads=[self.iota_d], writes=[io16])
        S.op('dve', lambda e: e.tensor_copy(out=idxf[:, :], in_=idx[:, :]), reads=[idx], writes=[idxf])
        S.op('dve', lambda e: e.tensor_scalar(out=idxf[:, :], in0=idxf[:, :], scalar1=float(NBLK), scalar2=None,
                                              op0=ALU.mult), reads=[idxf], writes=[idxf])
        S.op('dve', lambda e: e.tensor_tensor(out=idxaf[:, :, :], in0=idxf[:, :].unsqueeze(2).broadcast_to([128, 4, NBLK]),
                                              in1=io16[:, :].unsqueeze(1).broadcast_to([128, 4, NBLK]), op=ALU.add),
             reads=[idxf, io16], writes=[idxaf])
        S.op('dve', lambda e: e.tensor_copy(out=idxall[:, :, :], in_=idxaf[:, :, :]), reads=[idxaf], writes=[idxall])
        cblk = cache2d.t.rearrange("n (b e) -> (n b) e", e=R * 288)

        def attend_tile(n, kp_t, kp, va_t, va, masked):
            for g in range(2):
                self.transposes(kp_t, [kp[:, 8 * g + j, :] for j in range(8)], n, g, kT, kT[:, 8 * g:8 * g + 8, :n],
                                copy_eng='dve' if g == 0 else 'act')
            for h in range(16):
                S.op('pe', lambda e, h=h: e.matmul(ps[:n, 3, h * 4:(h + 1) * 4], lhsT=kT[:, h, :n], rhs=QTs[:, h, :],
                                                   start=True, stop=True), reads=[kT, QTs], writes=[psr[3]])
            S.op('act', lambda e: e.activation(out=PTs[:n, :], in_=ps[:n, 3, 0:64], func=AF.Exp, scale=scale),
                 reads=[psr[3]], writes=[PTs])
            if masked:
                S.op('dve', lambda e: e.tensor_tensor(out=PTs[:n, :], in0=PTs[:n, :], in1=mask4[:n, :], op=ALU.mult),
                     reads=[PTs, mask4], writes=[PTs])
            for h in range(16):
                b, c0 = 4 + h // 7, (h % 7) * 65
                S.op('pe', lambda e, h=h, b=b, c0=c0: e.matmul(ps[:4, b, c0:c0 + 65], lhsT=PTs[:n, h * 4:(h + 1) * 4],
                                                               rhs=va[:, h, :], start=True, stop=True),
                     reads=[PTs, va_t], writes=[psr[b]])
            S.op('dve', lambda e: e.tensor_tensor(out=acc[:, :, 0:455], in0=acc[:, :, 0:455], in1=ps[:4, 4:7, 0:455],
                                                  op=ALU.add), reads=[acc, psr[4], psr[5], psr[6]], writes=[acc])

        it = 0
        for s_ in range(4):
            r0 = SEQ + 4 * s_
            S.dma('sp', lambda e: e.dma_start(out=q4[:, :, :], in_=qpad_d[r0:r0 + 4, :].rearrange("p (h d) -> p h d", d=128)),
                  reads=[qpad_d], writes=[q4])
            S.dma('sp', lambda e: e.dma_start(out=k4[:, :, :], in_=kpad_d[r0:r0 + 4, :].rearrange("p (h d) -> p h d", d=128)),
                  reads=[kpad_d], writes=[k4])
            S.dma('sp', lambda e: e.dma_start(out=v4[:, :, :], in_=vaug_d[r0:r0 + 4, :].rearrange("p (h d) -> p h d", d=65)),
                  reads=[vaug_d], writes=[v4])
            for g in range(2):
                self.transposes(q4, [q4[:, 8 * g + j, :] for j in range(8)], 4, g, QTs, QTs[:, 8 * g:8 * g + 8, :],
                                copy_eng='dve')
            S.op('dve', lambda e: e.memset(acc[:, :, :], 0.0), writes=[acc])
            for rb in range(128 // R):
                pg = pgb[it % 2]
                it += 1
                S.dma('pool', lambda e, pg=pg, rb=rb: e.indirect_dma_start(
                    out=pg[:, :, :].rearrange("p r c -> p (r c)"), out_offset=None, in_=cblk,
                    in_offset=bass.IndirectOffsetOnAxis(ap=idxall[:, s_, rb:rb + 1], axis=0)),
                    reads=[cache2d, idxall], writes=[pg])
                for r in range(R):
                    self.expand_tile(128, pg, pg[:, r, :])
                    attend_tile(128, self.kpad, self.kpad[:, :, :], self.vaug, self.vaug[:, :, :], False)
            attend_tile(4, k4, k4[:, :, :], v4, v4[:, :, :], True)
            for b in range(3):
                nh = 7 if b < 2 else 2
                a3 = acc[:, b, 0:nh * 65].rearrange("p (h d) -> p h d", d=65)
                S.op('dve', lambda e, b=b, nh=nh, a3=a3: e.reciprocal(out=rec[:, 7 * b:7 * b + nh], in_=a3[:, :, 64]),
                     reads=[acc], writes=[rec])
                S.op('dve', lambda e, b=b, nh=nh, a3=a3: e.tensor_tensor(
                    out=o4[:, 7 * b * 64:(7 * b + nh) * 64].rearrange("p (h d) -> p h d", d=64), in0=a3[:, :, 0:64],
                    in1=rec[:, 7 * b:7 * b + nh].unsqueeze(2).broadcast_to([4, nh, 64]), op=ALU.mult),
                    reads=[acc, rec], writes=[o4])
            S.dma('sp', lambda e: e.dma_start(out=self.attn_s_d[4 * s_:4 * s_ + 4, :], in_=o4[:, :]), reads=[o4],
                  writes=[self.attn_s_d])
        S.barrier()
        ph.close()

    def phase_oproj(self, w_o_d, xdst):
        S = self.S
        ps, psr = self.ps, self.psr
        ph = contextlib.ExitStack()
        wo = self.sb('wo', [128, 8, D], BF16, ph)
        aT = self.sb('aT', [128, 8, 128], BF16, ph)
        xo = [self.sb('xo%d' % i, [128, D], F32, ph) for i in range(2)]
        S.dma('pool', lambda e: e.dma_start(out=wo[:, :, :], in_=w_o_d[:, :].rearrange("(k p) n -> p k n", p=128)),
              reads=[w_o_d], writes=[wo])
        S.dma('sp', lambda e: e.dma_start(out=self.attn[:NS, NT, :], in_=self.attn_s_d[:, :]), reads=[self.attn_s_d],
              writes=[self.attn])
        for ti in range(NT + 1):
            n = 128 if ti < NT else NS
            row0 = ti * 128
            md = self.pm if n == 128 else self.sm
            x = self.xb[ti % 2]
            S.dma('sp', lambda e: e.dma_start(out=x[:n, :], in_=self.xsrc[row0:row0 + n, :]), reads=[self.xsrc], writes=[x])
            self.transposes(self.attn, [self.attn[:n, ti, k * 128:(k + 1) * 128] for k in range(8)], n, 0, aT,
                            aT[:, :, :n])
            for c in range(2):
                for k in range(8):
                    S.op('pe', lambda e, c=c, k=k: e.matmul(ps[:n, 1 + c, :], lhsT=aT[:, k, :n],
                                                            rhs=wo[:, k, c * 512:(c + 1) * 512], start=(k == 0),
                                                            stop=(k == 7)), reads=[aT, wo], writes=[psr[1 + c]])
            o = xo[ti % 2]
            S.op('dve', lambda e: e.tensor_tensor(out=o[:n, :], in0=ps[:n, 1:3, :].rearrange("p b c -> p (b c)"),
                                                  in1=md[:n, 2, :], op=ALU.mult), reads=[psr[1], psr[2], md], writes=[o])
            S.op('dve', lambda e: e.tensor_tensor(out=o[:n, :], in0=o[:n, :], in1=x[:n, :], op=ALU.add),
                 reads=[o, x], writes=[o])
            S.dma('act', lambda e: e.dma_start(out=xdst[row0:row0 + n, :], in_=o[:n, :]), reads=[o], writes=[xdst])
        S.barrier()
        ph.close()

    def phase_peer(self, key, w_q_d, subT_d, u_d, v_d, xdst):
        S = self.S
        ps, psr = self.ps, self.psr
        ph = contextlib.ExitStack()
        wq = self.sb('pwq', [128, 8, D], BF16, ph)
        skb = self.sb('skb', [128, 8, 256], BF16, ph)
        qT = self.sb('pqT', [128, 8, 128], BF16, ph)
        s_sb = self.sb('s_sb', [128, 16, 128], F32, ph)
        s2 = self.sb('s2', [128, 16, 128], F32, ph)
        sv = self.sb('sv', [128, 16, 16], F32, ph)
        si = self.sb('si', [128, 16, 16], U32, ph)
        sif = self.sb('sif', [128, 16, 16], F32, ph)
        comb = self.sb('comb', [128, 8, 256], F32, ph)
        oh = self.sb('oh', [128, 8, 256], F32, ph)
        ts = self.sb('ts', [128, 8, 16], F32, ph)
        ts2 = self.sb('ts2', [128, 8, 16], F32, ph)
        tj = self.sb('tj', [128, 8, 16], U32, ph)
        tja = self.sb('tja', [128, 8, 16], U32, ph)
        tjb = self.sb('tjb', [128, 8, 16], U32, ph)
        af = self.sb('af', [128, 2, 128], F32, ph)
        ikjk = self.sb('ikjk', [128, 2, 128], F32, ph)
        eid = self.sb('eid', [128, 128], I32, ph)
        gs = self.sb('gs', [128, 16], F32, ph)
        adot = self.sb('adot', [128, 128], F32, ph)
        wgt = self.sb('wgt', [128, 128], BF16, ph)
        iota16 = self.sb('iota16', [128, 16], F32, ph)
        NB = 6
        GB = 4
        uvb = [self.sb('uvb%d' % i, [128, 2 * D], BF16, ph) for i in range(NB)]
        jb = self.sb('jb', [128, D], BF16, ph)
        Dg = [self.sb('Dg%d' % i, [128, GB, 128], BF16, ph) for i in range(2)]
        ad4 = [self.sb('ad4_%d' % i, [128, GB], F32, ph) for i in range(2)]
        ga4 = [self.sb('ga4_%d' % i, [128, GB], F32, ph) for i in range(2)]
        wg4 = [self.sb('wg4_%d' % i, [128, GB], BF16, ph) for i in range(2)]
        uv2d = u_d.t.rearrange("e t d -> e (t d)")
        xo = self.sb('pxo', [128, D], F32, ph)
        S.dma('pool', lambda e: e.dma_start(out=wq[:, :, :], in_=w_q_d[:, :].rearrange("(k p) n -> p k n", p=128)),
              reads=[w_q_d], writes=[wq])
        S.op('dve', lambda e: e.memset(skb[:, :, :], 0.0), writes=[skb])
        S.dma('pool', lambda e: e.dma_start(out=skb[0:64, :, 0:128], in_=subT_d[0:64, :, :]), reads=[subT_d], writes=[skb])
        S.dma('pool', lambda e: e.dma_start(out=skb[64:128, :, 128:256], in_=subT_d[64:128, :, :]), reads=[subT_d],
              writes=[skb])
        S.dma('sp', lambda e: e.dma_start(out=iota16[:, :], in_=self.iota_d[0:1, :].broadcast_to([128, 16])),
              reads=[self.iota_d], writes=[iota16])
        self.load_mod(key, 3)
        NEG = -1e30
        for ti in range(NT + 1):
            n = 128 if ti < NT else NS
            row0 = ti * 128
            md = self.pm if n == 128 else self.sm
            x = self.ada_tile(ti, n, row0)
            hT, hb = self.hT, self.hb
            for h in range(8):
                for k in range(8):
                    S.op('pe', lambda e, h=h, k=k: e.matmul(ps[:, 1 + h // 4, (h % 4) * 128:(h % 4) * 128 + n],
                                                            lhsT=wq[:, k, h * 128:(h + 1) * 128], rhs=hT[:, k, :n],
                                                            start=(k == 0), stop=(k == 7)),
                         reads=[wq, hT], writes=[psr[1 + h // 4]])
            S.op('act', lambda e: e.copy(out=qT[:, :, :n],
                                         in_=ps[:, 1:3, :].rearrange("p b (h c) -> p (b h) c", c=128)[:, :, :n]),
                 reads=[psr[1], psr[2]], writes=[qT])
            for h in range(8):
                S.op('pe', lambda e, h=h: e.matmul(ps[:n, 3 + h // 2, (h % 2) * 256:(h % 2) * 256 + 256],
                                                   lhsT=qT[:, h, :n], rhs=skb[:, h, :], start=True, stop=True),
                     reads=[qT, skb], writes=[psr[3 + h // 2]])
            S.op('act', lambda e: e.copy(out=s_sb[:n, :, :].rearrange("p g k -> p (g k)"),
                                         in_=ps[:n, 3:7, :].rearrange("p b c -> p (b c)")),
                 reads=[psr[3], psr[4], psr[5], psr[6]], writes=[s_sb])
            for g in range(16):
                S.op('dve', lambda e, g=g: e.max(out=sv[:n, g, 0:8], in_=s_sb[:n, g, :]), reads=[s_sb], writes=[sv])
            for g in range(16):
                S.op('dve', lambda e, g=g: e.max_index(out=si[:n, g, 0:8], in_max=sv[:n, g, 0:8], in_values=s_sb[:n, g, :]),
                     reads=[s_sb, sv], writes=[si])
            for g in range(16):
                S.op('dve', lambda e, g=g: e.match_replace(out=s2[:n, g, :], in_to_replace=sv[:n, g, 0:8],
                                                          in_values=s_sb[:n, g, :], imm_value=NEG),
                     reads=[s_sb, sv], writes=[s2])
            for g in range(16):
                S.op('dve', lambda e, g=g: e.max(out=sv[:n, g, 8:16], in_=s2[:n, g, :]), reads=[s2], writes=[sv])
            for g in range(16):
                S.op('dve', lambda e, g=g: e.max_index(out=si[:n, g, 8:16], in_max=sv[:n, g, 8:16], in_values=s2[:n, g, :]),
                     reads=[s2, sv], writes=[si])
            sv4 = sv[:n, :, :].rearrange("p (h t) k -> p h t k", t=2)
            comb4 = comb[:n, :, :].rearrange("p h (a b) -> p h a b", b=16)
            S.op('dve', lambda e: e.tensor_tensor(out=comb4, in0=sv4[:, :, 0, :].unsqueeze(3).broadcast_to([n, 8, 16, 16]),
                                                  in1=sv4[:, :, 1, :].unsqueeze(2).broadcast_to([n, 8, 16, 16]),
                                                  op=ALU.add), reads=[sv], writes=[comb])
            for h in range(8):
                S.op('dve', lambda e, h=h: e.max(out=ts[:n, h, 0:8], in_=comb[:n, h, :]), reads=[comb], writes=[ts])
            for h in range(8):
                S.op('dve', lambda e, h=h: e.max_index(out=tj[:n, h, 0:8], in_max=ts[:n, h, 0:8], in_values=comb[:n, h, :]),
                     reads=[comb, ts], writes=[tj])
            for h in range(8):
                S.op('dve', lambda e, h=h: e.match_replace(out=oh[:n, h, :], in_to_replace=ts[:n, h, 0:8],
                                                          in_values=comb[:n, h, :], imm_value=NEG),
                     reads=[comb, ts], writes=[oh])
            for h in range(8):
                S.op('dve', lambda e, h=h: e.max(out=ts[:n, h, 8:16], in_=oh[:n, h, :]), reads=[oh], writes=[ts])
            for h in range(8):
                S.op('dve', lambda e, h=h: e.max_index(out=tj[:n, h, 8:16], in_max=ts[:n, h, 8:16], in_values=oh[:n, h, :]),
                     reads=[oh, ts], writes=[tj])
            S.op('dve', lambda e: e.tensor_single_scalar(out=tja[:n, :, :], in_=tj[:n, :, :], scalar=4,
                                                         op=ALU.logical_shift_right), reads=[tj], writes=[tja])
            S.op('dve', lambda e: e.tensor_single_scalar(out=tjb[:n, :, :], in_=tj[:n, :, :], scalar=15,
                                                         op=ALU.bitwise_and), reads=[tj], writes=[tjb])
            S.op('dve', lambda e: e.tensor_copy(out=af[:n, 0, :], in_=tja[:n, :, :].rearrange("p h k -> p (h k)")),
                 reads=[tja], writes=[af])
            S.op('dve', lambda e: e.tensor_copy(out=af[:n, 1, :], in_=tjb[:n, :, :].rearrange("p h k -> p (h k)")),
                 reads=[tjb], writes=[af])
            S.op('dve', lambda e: e.tensor_copy(out=sif[:n, :, :], in_=si[:n, :, :]), reads=[si], writes=[sif])
            sif4 = sif[:n, :, :].rearrange("p (h t) k -> p h t k", t=2)
            io4 = iota16[:n, :].unsqueeze(1).unsqueeze(1).broadcast_to([n, 8, 16, 16])
            for t in range(2):
                a4 = af[:n, t, :].rearrange("p (h k) -> p h k", k=16).unsqueeze(3).broadcast_to([n, 8, 16, 16])
                oh4 = oh[:n, :, :].rearrange("p h (k a) -> p h k a", a=16)
                S.op('dve', lambda e, a4=a4, oh4=oh4: e.tensor_tensor(out=oh4, in0=a4, in1=io4, op=ALU.is_equal),
                     reads=[af, iota16], writes=[oh])
                S.op('dve', lambda e, t=t, oh4=oh4: e.tensor_tensor(
                    out=oh4, in0=oh4, in1=sif4[:, :, t, :].unsqueeze(2).broadcast_to([n, 8, 16, 16]), op=ALU.mult),
                    reads=[oh, sif], writes=[oh])
                S.op('dve', lambda e, t=t, oh4=oh4: e.tensor_reduce(
                    out=ikjk[:n, t, :].rearrange("p (h k) -> p h k", k=16), in_=oh4, axis=AX.X, op=ALU.add),
                    reads=[oh], writes=[ikjk])
            S.op('dve', lambda e: e.scalar_tensor_tensor(out=af[:n, 0, :], in0=ikjk[:n, 0, :], scalar=128.0,
                                                         in1=ikjk[:n, 1, :], op0=ALU.mult, op1=ALU.add),
                 reads=[ikjk], writes=[af])
            S.op('dve', lambda e: e.tensor_copy(out=eid[:n, :], in_=af[:n, 0, :]), reads=[af], writes=[eid])
            S.op('dve', lambda e: e.tensor_tensor(out=ts2[:n, :, :], in0=ts[:n, :, :],
                                                  in1=ts[:n, :, 0:1].broadcast_to([n, 8, 16]), op=ALU.subtract),
                 reads=[ts], writes=[ts2])
            S.op('act', lambda e: e.activation(out=ts2[:n, :, :], in_=ts2[:n, :, :], func=AF.Exp), reads=[ts2], writes=[ts2])
            S.op('dve', lambda e: e.tensor_reduce(out=gs[:n, 0:8], in_=ts2[:n, :, :], axis=AX.X, op=ALU.add),
                 reads=[ts2], writes=[gs])
            S.op('dve', lambda e: e.reciprocal(out=gs[:n, 8:16], in_=gs[:n, 0:8]), reads=[gs], writes=[gs])
            S.op('dve', lambda e: e.tensor_tensor(out=ts2[:n, :, :], in0=ts2[:n, :, :],
                                                  in1=gs[:n, 8:16].unsqueeze(2).broadcast_to([n, 8, 16]), op=ALU.mult),
                 reads=[ts2, gs], writes=[ts2])
            gflat = ts2[:n, :, :].rearrange("p h k -> p (h k)")
            for bi in range(128 // GB):
                pb = bi % 2
                a_, g_, w_, D_ = ad4[pb], ga4[pb], wg4[pb], Dg[pb]
                for j in range(GB):
                    m = bi * GB + j
                    buf = uvb[m % NB]
                    S.dma('pool', lambda e, buf=buf, m=m: e.indirect_dma_start(
                        out=buf[:n, :], out_offset=None, in_=uv2d,
                        in_offset=bass.IndirectOffsetOnAxis(ap=eid[:n, m:m + 1], axis=0)), reads=[u_d, eid], writes=[buf])
                    S.op('dve', lambda e, buf=buf, j=j: e.scalar_tensor_tensor(
                        out=jb[:n, :], in0=hb[:n, :], scalar=1.0, in1=buf[:n, 0:D], op0=ALU.mult, op1=ALU.mult,
                        accum_out=a_[:n, j:j + 1]), reads=[hb, buf], writes=[jb, a_])
                S.op('act', lambda e: e.activation(out=g_[:n, :], in_=a_[:n, :], func=AF.Gelu), reads=[a_], writes=[g_])
                S.op('dve', lambda e, bi=bi: e.tensor_tensor(out=w_[:n, :], in0=g_[:n, :],
                                                             in1=gflat[:, bi * GB:(bi + 1) * GB], op=ALU.mult),
                     reads=[g_, ts2], writes=[w_])
                S.op('dve', lambda e: e.tensor_tensor(
                    out=D_[:n, :, :n], in0=self.ident[:n, :n].unsqueeze(1).broadcast_to([n, GB, n]),
                    in1=w_[:n, :].unsqueeze(2).broadcast_to([n, GB, n]), op=ALU.mult),
                    reads=[self.ident, w_], writes=[D_])
                for j in range(GB):
                    m = bi * GB + j
                    buf = uvb[m % NB]
                    for c in range(2):
                        S.op('pe', lambda e, buf=buf, m=m, c=c, j=j: e.matmul(
                            ps[:n, 1 + c, :], lhsT=D_[:n, j, :n], rhs=buf[:n, D + c * 512:D + (c + 1) * 512],
                            start=(m == 0), stop=(m == 127)), reads=[D_, buf], writes=[psr[1 + c]])
            S.op('dve', lambda e: e.tensor_tensor(out=xo[:n, :], in0=ps[:n, 1:3, :].rearrange("p b c -> p (b c)"),
                                                  in1=md[:n, 2, :], op=ALU.mult), reads=[psr[1], psr[2], md], writes=[xo])
            S.op('dve', lambda e: e.tensor_tensor(out=xo[:n, :], in0=xo[:n, :], in1=x[:n, :], op=ALU.add),
                 reads=[xo, x], writes=[xo])
            S.dma('act', lambda e: e.dma_start(out=xdst[row0:row0 + n, :], in_=xo[:n, :]), reads=[xo], writes=[xdst])
        S.barrier()
        ph.close()

    def dil_norm_rope(self, raw, raw_res, gain, tb, n, dst_t, dst):
        S = self.S
        st, kf, tb_t = self.stD, self.kfD, self.rt_all
        S.op('act', lambda e: e.activation(out=self.junk[:n, 0:1024], in_=raw, func=AF.Square), reads=raw_res,
             writes=[self.junk])
        S.op('dve', lambda e: e.tensor_reduce(out=st[:n, 0:8], in_=self.junk[:n, 0:1024].rearrange("p (h d) -> p h d", d=128),
                                              axis=AX.X, op=ALU.add), reads=[self.junk], writes=[st])
        self.rstd((st, st[:n, 0:8]), (st, st[:n, 8:16]), n, 1.0 / 128)
        r3 = raw.rearrange("p (h d) -> p h d", d=128)
        k3 = kf[:n, :].rearrange("p (h d) -> p h d", d=128)
        d3 = dst.rearrange("p (h d) -> p h d", d=128)
        S.op('dve', lambda e: e.tensor_tensor(out=k3, in0=r3, in1=st[:n, 8:16].unsqueeze(2).broadcast_to([n, 8, 128]),
                                              op=ALU.mult), reads=list(raw_res) + [st], writes=[kf])
        S.op('dve', lambda e: e.tensor_tensor(out=d3, in0=k3, in1=gain[:n, :].unsqueeze(1).broadcast_to([n, 8, 128]),
                                              op=ALU.mult), reads=[kf, gain], writes=[dst_t])
        self.rope(dst_t, d3[:, :, 0:32], dst_t, d3[:, :, 0:32], tb, n, 8)

    def phase_KV(self, kv_w, kv_g_k, rt, dil_p, dil_s, kd, vd):
        S = self.S
        ps, psr = self.ps, self.psr
        ph = contextlib.ExitStack()
        self.stD = self.sb('stD', [128, 16], F32, ph)
        self.kfD = self.sb('kfD', [128, D], F32, ph)
        t32 = self.sb('t32d', [128, 16, 32], F32, ph)
        t32b = self.sb('t32bd', [128, 16, 32], F32, ph)
        self._mk_rope(t32, t32b)
        gk = self.sb('gk', [128, 3, 128], F32, ph)
        for g in range(3):
            S.dma('sp', lambda e, g=g: e.dma_start(out=gk[:, g, :], in_=kv_g_k[g:g + 1, :].broadcast_to([128, 128])),
                  reads=[kv_g_k], writes=[gk])
        wkv = [self.sb('wkv%d' % i, [128, 8, 1024], BF16, ph) for i in range(2)]
        of = [self.sb('kvof%d' % i, [128, D], F32, ph) for i in range(2)]
        ob = [self.sb('kvob%d' % i, [128, D], BF16, ph) for i in range(2)]
        hTall = self.attn.t[:, :, :].rearrange("p t d -> p (t d)").rearrange("p (k c) -> p k c", k=8)
        self.load_mod('kv', 2)
        for ti in range(NT + 1):
            n = 128 if ti < NT else NS
            row0 = ti * 128
            self.ada_tile(ti, n, row0)
            S.op('act', lambda e: e.copy(out=hTall[:, :, row0:row0 + n], in_=self.hT[:, :, :n]), reads=[self.hT],
                 writes=[self.attn])
        it = 0
        for cg in range(6):
            two, g = cg // 3, cg % 3
            W = (128, 512, 2048)[g]
            w_ = wkv[cg % 2]
            S.dma('pool', lambda e, w_=w_: e.dma_start(
                out=w_[:, :, :], in_=kv_w[:, cg * 1024:(cg + 1) * 1024].rearrange("(k p) n -> p k n", p=128)),
                reads=[kv_w], writes=[w_])
            for ti in range(NT + 1):
                n = 128 if ti < NT else NS
                row0 = ti * 128
                b0 = 1 + 2 * (it % 2)
                o_f, o_b = of[it % 2], ob[it % 2]
                it += 1
                for c in range(2):
                    for k in range(8):
                        S.op('pe', lambda e, c=c, k=k: e.matmul(ps[:n, b0 + c, :], lhsT=hTall[:, k, row0:row0 + n],
                                                                rhs=w_[:, k, c * 512:(c + 1) * 512], start=(k == 0),
                                                                stop=(k == 7)), reads=[self.attn, w_], writes=[psr[b0 + c]])
                raw = ps[:n, b0:b0 + 2, :].rearrange("p b c -> p (b c)")
                if two == 0:
                    self.dil_norm_rope(raw, [psr[b0], psr[b0 + 1]], gk_g(gk, g), self.rt_all_tile(ti), n, o_f, o_f[:n, :])
                else:
                    S.op('act', lambda e: e.copy(out=o_f[:n, :], in_=raw), reads=[psr[b0], psr[b0 + 1]], writes=[o_f])
                S.op('act', lambda e: e.copy(out=o_b[:n, :], in_=o_f[:n, :]), reads=[o_f], writes=[o_b])
                dst_b = kd if two == 0 else vd
                S.dma('sp', lambda e: e.dma_start(out=dst_b[g, row0:row0 + n, :], in_=o_b[:n, :]), reads=[o_b],
                      writes=[dst_b])
                if ti < NT:
                    lo = SEQ - W
                    if row0 >= lo:
                        S.dma('act', lambda e: e.dma_start(out=dil_p[g][row0 - lo:row0 - lo + n, two, :], in_=o_f[:n, :]),
                              reads=[o_f], writes=[dil_p[g]])
                else:
                    for s_ in range(4):
                        S.dma('act', lambda e, s_=s_: e.dma_start(out=dil_s[g][s_, W - 4:W, two, :],
                                                                  in_=o_f[4 * s_:4 * s_ + 4, :]), reads=[o_f],
                              writes=[dil_s[g]])
        S.barrier()
        ph.close()

    def phase_B1(self, b_w_q, b_g_q, qd):
        S = self.S
        ps, psr = self.ps, self.psr
        ph = contextlib.ExitStack()
        self.stD = self.sb('stD1', [128, 16], F32, ph)
        self.kfD = self.sb('kfD1', [128, D], F32, ph)
        t32 = self.sb('t32e', [128, 16, 32], F32, ph)
        t32b = self.sb('t32be', [128, 16, 32], F32, ph)
        self._mk_rope(t32, t32b)
        gq = self.sb('gq', [128, 3, 128], F32, ph)
        for g in range(3):
            S.dma('sp', lambda e, g=g: e.dma_start(out=gq[:, g, :], in_=b_g_q[g:g + 1, :].broadcast_to([128, 128])),
                  reads=[b_g_q], writes=[gq])
        wq = self.sb('bwq', [128, 8, 3072], BF16, ph)
        of = [self.sb('qof%d' % i, [128, D], F32, ph) for i in range(2)]
        ob = [self.sb('qob%d' % i, [128, D], BF16, ph) for i in range(2)]
        for c in range(3):
            S.dma('pool', lambda e, c=c: e.dma_start(
                out=wq[:, :, c * 1024:(c + 1) * 1024],
                in_=b_w_q[:, c * 1024:(c + 1) * 1024].rearrange("(k p) n -> p k n", p=128)), reads=[b_w_q], writes=[wq])
        self.load_mod('b', 3)
        it = 0
        for ti in range(NT + 1):
            n = 128 if ti < NT else NS
            row0 = ti * 128
            self.ada_tile(ti, n, row0)
            for c in range(6):
                for k in range(8):
                    S.op('pe', lambda e, c=c, k=k: e.matmul(ps[:n, 1 + c, :], lhsT=self.hT[:, k, :n],
                                                            rhs=wq[:, k, c * 512:(c + 1) * 512], start=(k == 0),
                                                            stop=(k == 7)), reads=[self.hT, wq], writes=[psr[1 + c]])
            for g in range(3):
                o_f, o_b = of[it % 2], ob[it % 2]
                it += 1
                raw = ps[:n, 1 + 2 * g:3 + 2 * g, :].rearrange("p b c -> p (b c)")
                self.dil_norm_rope(raw, [psr[1 + 2 * g], psr[2 + 2 * g]], gk_g(gq, g), self.rt_all_tile(ti), n, o_f,
                                   o_f[:n, :])
                S.op('act', lambda e: e.copy(out=o_b[:n, :], in_=o_f[:n, :]), reads=[o_f], writes=[o_b])
                S.dma('sp', lambda e, g=g: e.dma_start(out=qd[g, row0:row0 + n, :], in_=o_b[:n, :]), reads=[o_b], writes=[qd])
        S.barrier()
        ph.close()

    def phase_B2(self, qd, kd, vd, dacc, mask2_f):
        S = self.S
        ps, psr = self.ps, self.psr
        ph = contextlib.ExitStack()
        qt = self.sb('dq', [128, 8, 128], BF16, ph)
        kt = [self.sb('dk%d' % i, [128, 8, 128], BF16, ph) for i in range(2)]
        vt = [self.sb('dv%d' % i, [128, 8, 129], BF16, ph) for i in range(2)]
        QT = self.sb('dQT', [128, 8, 128], BF16, ph)
        KT = [self.sb('dKT%d' % i, [128, 8, 128], BF16, ph) for i in range(2)]
        PT = [self.sb('dPT%d' % i, [128, 512], BF16, ph) for i in range(2)]
        mask2 = self.sb('mask2', [128, 512], BF16, ph)
        oacc = [self.sb('doa%d' % i, [128, 8, 129], F32, ph) for i in range(2)]
        S.dma('pool', lambda e: e.dma_start(out=mask2[:, :], in_=mask2_f[:, :]), reads=[mask2_f], writes=[mask2])
        for i in range(2):
            S.op('pool', lambda e, i=i: e.memset(vt[i][:, :, :], 1.0), writes=[vt[i]])
        scale = 128.0 ** -0.5
        cnt = 0
        tcount = 0
        for g in range(3):
            dil = (1, 4, 16)[g]
            nblk = SEQ // dil // 128
            for r in range(dil):
                for I in range(nblk):
                    cur = tcount % 2
                    prv = 1 - cur
                    tcount += 1
                    t0 = r + dil * 128 * I
                    rows = lambda T: T[g, t0:t0 + dil * 127 + 1:dil, :].rearrange("p (h d) -> p h d", d=128)
                    S.dma('sp', lambda e: e.dma_start(out=qt[:, :, :], in_=rows(qd)), reads=[qd], writes=[qt])
                    S.dma('act', lambda e: e.dma_start(out=kt[cur][:, :, :], in_=rows(kd)), reads=[kd], writes=[kt[cur]])
                    S.dma('sp', lambda e: e.dma_start(out=vt[cur][:, :, 0:128], in_=rows(vd)), reads=[vd], writes=[vt[cur]])
                    self.transposes(qt, [qt[:, h, :] for h in range(8)], 128, 0, QT, QT[:, :, :], copy_eng='dve')
                    self.transposes(kt[cur], [kt[cur][:, h, :] for h in range(8)], 128, 0, KT[cur], KT[cur][:, :, :],
                                    copy_eng='act')
                    oa = oacc[cur]
                    for hp in range(4):
                        sb_ = 1 + (cnt % 2)
                        pt = PT[cnt % 2]
                        cnt += 1
                        for hh in range(2):
                            h = 2 * hp + hh
                            if I > 0:
                                S.op('pe', lambda e, h=h, hh=hh: e.matmul(ps[:, sb_, hh * 256:hh * 256 + 128],
                                                                         lhsT=KT[prv][:, h, :], rhs=QT[:, h, :],
                                                                         start=True, stop=True),
                                     reads=[KT[prv], QT], writes=[psr[sb_]])
                            S.op('pe', lambda e, h=h, hh=hh: e.matmul(ps[:, sb_, hh * 256 + 128:hh * 256 + 256],
                                                                     lhsT=KT[cur][:, h, :], rhs=QT[:, h, :],
                                                                     start=True, stop=True),
                                 reads=[KT[cur], QT], writes=[psr[sb_]])
                        if I > 0:
                            S.op('act', lambda e: e.activation(out=pt[:, :], in_=ps[:, sb_, :], func=AF.Exp, scale=scale),
                                 reads=[psr[sb_]], writes=[pt])
                        else:
                            for hh in range(2):
                                S.op('act', lambda e, hh=hh: e.activation(
                                    out=pt[:, hh * 256 + 128:hh * 256 + 256], in_=ps[:, sb_, hh * 256 + 128:hh * 256 + 256],
                                    func=AF.Exp, scale=scale), reads=[psr[sb_]], writes=[pt])
                        S.op('dve', lambda e: e.tensor_tensor(out=pt[:, :], in0=pt[:, :], in1=mask2[:, :], op=ALU.mult),
                             reads=[pt, mask2], writes=[pt])
                        for hh in range(2):
                            h = 2 * hp + hh
                            ob_, c0 = 4 + h // 3, (h % 3) * 129
                            if I > 0:
                                S.op('pe', lambda e, h=h, hh=hh, ob_=ob_, c0=c0: e.matmul(
                                    ps[:, ob_, c0:c0 + 129], lhsT=pt[:, hh * 256:hh * 256 + 128], rhs=vt[prv][:, h, :],
                                    start=True, stop=False), reads=[pt, vt[prv]], writes=[psr[ob_]])
                            S.op('pe', lambda e, h=h, hh=hh, ob_=ob_, c0=c0: e.matmul(
                                ps[:, ob_, c0:c0 + 129], lhsT=pt[:, hh * 256 + 128:hh * 256 + 256], rhs=vt[cur][:, h, :],
                                start=(I == 0), stop=True), reads=[pt, vt[cur]], writes=[psr[ob_]])
                    for b in range(3):
                        nh = 3 if b < 2 else 2
                        S.op('dve', lambda e, b=b, nh=nh: e.tensor_copy(
                            out=oa[:, 3 * b:3 * b + nh, :].rearrange("p h d -> p (h d)"), in_=ps[:, 4 + b, 0:nh * 129]),
                            reads=[psr[4 + b]], writes=[oa])
                    S.dma('act', lambda e: e.dma_start(out=dacc[g, t0:t0 + dil * 127 + 1:dil, :],
                                                       in_=oa[:, :, :].rearrange("p h d -> p (h d)")), reads=[oa],
                          writes=[dacc])
        cb = [self.sb('dcb%d' % i, [128, 3, 8 * 129], F32, ph) for i in range(2)]
        rc = self.sb('drc', [128, 8], F32, ph)
        for ti in range(NT):
            c_ = cb[ti % 2]
            for g in range(3):
                S.dma('sp', lambda e, g=g: e.dma_start(out=c_[:, g, :], in_=dacc[g, ti * 128:(ti + 1) * 128, :]),
                      reads=[dacc], writes=[c_])
            S.op('dve', lambda e: e.tensor_tensor(out=c_[:, 0, :], in0=c_[:, 0, :], in1=c_[:, 1, :], op=ALU.add),
                 reads=[c_], writes=[c_])
            S.op('dve', lambda e: e.tensor_tensor(out=c_[:, 0, :], in0=c_[:, 0, :], in1=c_[:, 2, :], op=ALU.add),
                 reads=[c_], writes=[c_])
            c3 = c_[:, 0, :].rearrange("p (h d) -> p h d", d=129)
            S.op('dve', lambda e: e.reciprocal(out=rc[:, :], in_=c3[:, :, 128]), reads=[c_], writes=[rc])
            S.op('dve', lambda e: e.tensor_tensor(out=self.attn[:, ti, :].rearrange("p (h d) -> p h d", d=128),
                                                  in0=c3[:, :, 0:128], in1=rc[:, :].unsqueeze(2).broadcast_to([128, 8, 128]),
                                                  op=ALU.mult), reads=[c_, rc], writes=[self.attn])
        S.barrier()
        ph.close()

    def phase_B3(self, qd, cache_dil, dil_s, bd_f):
        S = self.S
        ps, psr = self.ps, self.psr
        ph = contextlib.ExitStack()
        kc = [self.sb('skc%d' % i, [128, D], F32, ph) for i in range(2)]
        vc = [self.sb('svc%d' % i, [128, D + 8], F32, ph) for i in range(2)]
        kn = [self.sb('skn%d' % i, [4, D], F32, ph) for i in range(2)]
        vn = [self.sb('svn%d' % i, [4, D + 8], F32, ph) for i in range(2)]
        qb = [self.sb('sqb%d' % i, [128, D], BF16, ph) for i in range(2)]
        pr = self.sb('spr', [128, D], F32, ph)
        sc = self.sb('ssc', [128, 16], F32, ph)
        P = [self.sb('sP%d' % i, [128, 16], F32, ph) for i in range(2)]
        o8 = self.sb('so8', [8, D + 8], F32, ph)
        bd = self.sb('sbd', [8, D + 8], F32, ph)
        ones8 = self.sb('sones', [8, 1], F32, ph)
        row = self.sb('srow', [1, D + 8], F32, ph)
        rrec = self.sb('srrec', [1, 8], F32, ph)
        orow = self.sb('sorow', [1, D], BF16, ph)
        S.dma('sp', lambda e: e.dma_start(out=bd[:, :], in_=bd_f[:, :]), reads=[bd_f], writes=[bd])
        S.op('dve', lambda e: e.memset(ones8[:, :], 1.0), writes=[ones8])
        for i in range(2):
            S.op('dve', lambda e, i=i: e.memset(vc[i][:, D:D + 8], 1.0), writes=[vc[i]])
            S.op('dve', lambda e, i=i: e.memset(vn[i][:, D:D + 8], 1.0), writes=[vn[i]])
        scale = 128.0 ** -0.5
        it = 0
        for s_ in range(4):
            for t in range(4):
                tok = SEQ + 4 * s_ + t
                nmm = 0
                for g in range(3):
                    dil = (1, 4, 16)[g]
                    W = (128, 512, 2048)[g]
                    Mc = 128 if dil > 1 else 128 - t
                    n0, nn = (t, 1) if dil > 1 else (0, t + 1)
                    b = it % 2
                    it += 1
                    kc_, vc_, kn_, vn_, qb_, P_ = kc[b], vc[b], kn[b], vn[b], qb[b], P[b]
                    cd = cache_dil[g]
                    S.dma('sp', lambda e: e.dma_start(out=kc_[:Mc, :], in_=cd[s_, t:t + dil * (Mc - 1) + 1:dil, 0, :]),
                          reads=[cd], writes=[kc_])
                    S.dma('act', lambda e: e.dma_start(out=vc_[:Mc, 0:D], in_=cd[s_, t:t + dil * (Mc - 1) + 1:dil, 1, :]),
                          reads=[cd], writes=[vc_])
                    S.dma('sp', lambda e: e.dma_start(out=kn_[:nn, :], in_=dil_s[g][s_, W - 4 + n0:W - 4 + n0 + nn, 0, :]),
                          reads=[dil_s[g]], writes=[kn_])
                    S.dma('act', lambda e: e.dma_start(out=vn_[:nn, 0:D], in_=dil_s[g][s_, W - 4 + n0:W - 4 + n0 + nn, 1, :]),
                          reads=[dil_s[g]], writes=[vn_])
                    S.dma('sp', lambda e: e.dma_start(out=qb_[:, :], in_=qd[g, tok:tok + 1, :].broadcast_to([128, D])),
                          reads=[qd], writes=[qb_])
                    for (src, m, col) in ((kc_, Mc, 0), (kn_, nn, 8)):
                        S.op('dve', lambda e, src=src, m=m: e.tensor_tensor(out=pr[:m, :], in0=src[:m, :], in1=qb_[:m, :],
                                                                            op=ALU.mult), reads=[src, qb_], writes=[pr])
                        S.op('dve', lambda e, m=m, col=col: e.tensor_reduce(
                            out=sc[:m, col:col + 8], in_=pr[:m, :].rearrange("p (h d) -> p h d", d=128), axis=AX.X,
                            op=ALU.add), reads=[pr], writes=[sc])
                        S.op('act', lambda e, m=m, col=col: e.activation(out=P_[:m, col:col + 8], in_=sc[:m, col:col + 8],
                                                                        func=AF.Exp, scale=scale), reads=[sc], writes=[P_])
                    for (pv, m, col) in ((vc_, Mc, 0), (vn_, nn, 8)):
                        for c, (c0, cw) in enumerate(((0, 512), (512, 512), (1024, 8))):
                            S.op('pe', lambda e, pv=pv, m=m, col=col, c=c, c0=c0, cw=cw: e.matmul(
                                ps[:8, 1 + c, 0:cw], lhsT=P_[:m, col:col + 8], rhs=pv[:m, c0:c0 + cw],
                                start=(nmm == 0), stop=(nmm == 5)), reads=[P_, pv], writes=[psr[1 + c]])
                        nmm += 1
                S.op('dve', lambda e: e.tensor_tensor(out=o8[:, 0:1024], in0=ps[:8, 1:3, :].rearrange("p b c -> p (b c)"),
                                                      in1=bd[:, 0:1024], op=ALU.mult), reads=[psr[1], psr[2], bd], writes=[o8])
                S.op('dve', lambda e: e.tensor_tensor(out=o8[:, 1024:1032], in0=ps[:8, 3, 0:8], in1=bd[:, 1024:1032],
                                                      op=ALU.mult), reads=[psr[3], bd], writes=[o8])
                for c, (c0, cw) in enumerate(((0, 512), (512, 512), (1024, 8))):
                    S.op('pe', lambda e, c=c, c0=c0, cw=cw: e.matmul(ps[:1, 4 + c, 0:cw], lhsT=ones8[:, :],
                                                                     rhs=o8[:, c0:c0 + cw], start=True, stop=True),
                         reads=[ones8, o8], writes=[psr[4 + c]])
                S.op('act', lambda e: e.copy(out=row[:, 0:1024], in_=ps[:1, 4:6, :].rearrange("p b c -> p (b c)")),
                     reads=[psr[4], psr[5]], writes=[row])
                S.op('dve', lambda e: e.reciprocal(out=rrec[:, :], in_=ps[:1, 6, 0:8]), reads=[psr[6]], writes=[rrec])
                S.op('dve', lambda e: e.tensor_tensor(out=orow[:, :].rearrange("p (h d) -> p h d", d=128),
                                                      in0=row[:, 0:1024].rearrange("p (h d) -> p h d", d=128),
                                                      in1=rrec[:, :].unsqueeze(2).broadcast_to([1, 8, 128]), op=ALU.mult),
                     reads=[row, rrec], writes=[orow])
                S.dma('sp', lambda e: e.dma_start(out=self.attn_s_d[4 * s_ + t:4 * s_ + t + 1, :], in_=orow[:, :]),
                      reads=[orow], writes=[self.attn_s_d])
        S.barrier()
        ph.close()

    def phase_convert(self, pairs):
        S = self.S
        ph = contextlib.ExitStack()
        RB = 8
        bufs = [self.sb('cvb%d' % i, [128, RB, D], BF16, ph) for i in range(3)]
        it = 0
        for src, dst, half in pairs:
            sv = src.t.rearrange("(p r) d -> p r d", p=128)
            dv = dst.t[:, half, :].rearrange("(p r) d -> p r d", p=128)
            for r0 in range(0, 128, RB):
                b_ = bufs[it % 3]
                S.dma('pool', lambda e, b_=b_, sv=sv, r0=r0: e.dma_start(out=b_[:, :, :], in_=sv[:, r0:r0 + RB, :]),
                      reads=[src], writes=[b_])
                S.dma('sp' if it % 2 == 0 else 'act',
                      lambda e, b_=b_, dv=dv, r0=r0: e.dma_start(out=dv[:, r0:r0 + RB, :], in_=b_[:, :, :]),
                      reads=[b_], writes=[dst])
                it += 1
        S.barrier()
        ph.close()

    def finish(self):
        self.S.finish()
        if self.phA is not None:
            self.phA.close()
        self.stack.close()
        return self.nc


def _rope_table():
    half = 16
    inv = (np.float32(500000.0) ** (-np.arange(half, dtype=np.float32) / np.float32(half))).astype(np.float32)
    pos = np.concatenate([np.arange(SEQ), PAST + (np.arange(NS) % 4)]).astype(np.float32)
    ang = (pos[:, None] * inv[None, :]).astype(np.float32)
    c, s_ = np.cos(ang).astype(np.float32), np.sin(ang).astype(np.float32)
    return np.ascontiguousarray(np.concatenate([c, c, -s_, s_], axis=1))


def _mask2():
    kk = np.arange(128)[:, None]
    qq = np.arange(128)[None, :]
    prev = (kk >= qq).astype(np.float32)
    cur = (kk <= qq).astype(np.float32)
    return np.ascontiguousarray(np.concatenate([prev, cur, prev, cur], axis=1))


def _bd():
    m = np.zeros((8, D + 8), np.float32)
    for h in range(8):
        m[h, h * 128:(h + 1) * 128] = 1.0
        m[h, D + h] = 1.0
    return m


def _prep(inp):
    f = lambda a: np.ascontiguousarray(np.asarray(a, dtype=np.float32))
    shared = {
        'rt': _rope_table(), 'identf': np.eye(128, dtype=np.float32),
        'maskd_f': np.triu(np.ones((128, 128), np.float32)),
        'mask4_f': np.ascontiguousarray(np.tile((np.arange(4)[:, None] <= np.arange(4)[None, :]).astype(np.float32), (1, 16))),
        'cache_mla': f(inp['cache_mla'][0].reshape(5120, 128 * 288)), 'a_w_o': f(inp['a_w_o'][0]),
        'iota_d': np.arange(16, dtype=np.float32)[None, :],
        'mask2_f': _mask2(), 'bd_f': _bd(),
        'kv_w': f(inp['kv_w']), 'kv_g_k': f(inp['kv_g_k']), 'b_w_q': f(inp['b_w_q'][0]), 'b_g_q': f(inp['b_g_q'][0]),
        'b_w_o': f(inp['b_w_o'][0]),
        'a_mod_w': f(inp['a_mod_w'][0]), 'f_mod_w0': f(inp['f_mod_w'][0]), 'kv_mod_w': f(inp['kv_mod_w']),
        'b_mod_w': f(inp['b_mod_w'][0]), 'f_mod_w1': f(inp['f_mod_w'][1]),
        'a_mod_b': f(inp['a_mod_b'][0:1]), 'f_mod_b0': f(inp['f_mod_b'][0:1]), 'kv_mod_b': f(inp['kv_mod_b'][None]),
        'b_mod_b': f(inp['b_mod_b'][0:1]), 'f_mod_b1': f(inp['f_mod_b'][1:2]),
        'a_w_dq': f(inp['a_w_dq'][0]), 'a_w_uq': f(inp['a_w_uq'][0]), 'a_w_dkv': f(inp['a_w_dkv'][0]),
        'a_w_uk': f(inp['a_w_uk'][0].reshape(256, 1024)), 'a_w_uv': f(inp['a_w_uv'][0].reshape(256, 1024)),
    }
    for l in range(2):
        shared['f_w_q%d' % l] = f(inp['f_w_q'][l])
        shared['f_subT%d' % l] = f(np.asarray(inp['f_subkeys'][l]).transpose(1, 3, 0, 2).reshape(128, 8, 128))
        shared['f_u%d' % l] = f(inp['f_u'][l])
        shared['f_v%d' % l] = f(inp['f_v'][l])
    for k in ('a_g_cq', 'a_g_ckv', 'a_g_qn', 'a_g_qr', 'a_g_kr', 'a_g_kn'):
        shared[k] = f(inp[k][0:1])
    maps = []
    for c in range(NCORES):
        m = dict(shared)
        xs = np.asarray(inp['x_sample'], np.float32)[4 * c:4 * c + 4].reshape(NS, D)
        m['xin'] = np.ascontiguousarray(np.concatenate([np.asarray(inp['x_prompt'], np.float32)[c], xs], axis=0))
        c5 = np.concatenate([np.asarray(inp['c_prompt'], np.float32)[c:c + 1],
                             np.asarray(inp['c_sample'], np.float32)[4 * c:4 * c + 4]], axis=0)
        m['cT'] = np.ascontiguousarray(c5.reshape(5, 8, 128).transpose(2, 1, 0).reshape(128, 40))
        for nm in ('cache_dil0', 'cache_dil1', 'cache_dil2'):
            cd = np.asarray(inp[nm], np.float32)[4 * c:4 * c + 4]
            m[nm] = np.ascontiguousarray(cd.reshape(4, cd.shape[1], 2, D))
        m['ptT'] = np.ascontiguousarray(np.asarray(inp['page_table'], np.int32)[4 * c:4 * c + 4].T)
        maps.append(m)
    return maps


def run(inp, upto='all', debug=False, cores=None):
    b = Builder(upto=upto, debug=debug)
    nc = b.build()
    print('instructions:', b.S.ninstr, {k: v for k, v in b.S.cnt.items()}, b.S.dcnt)
    maps = _prep(inp)
    used = set(b.dram.keys())
    maps = [{k: v for k, v in m.items() if k in used} for m in maps]
    if cores is not None:
        maps = [maps[c] for c in cores]
    res = run_bass_kernel_spmd(nc, maps, core_ids=list(range(len(maps))))
    return res.results


def kernel(**inputs):
    res = run(inputs)
    Ws = (128, 512, 2048)
    y = np.stack([np.asarray(r['y']) for r in res])
    ml = np.stack([np.asarray(r['mla_rows']) for r in res])
    outs = [np.ascontiguousarray(y[:, :SEQ]), np.ascontiguousarray(y[:, SEQ:].reshape(32, 4, D)),
            np.ascontiguousarray(ml[:, :SEQ])[None], np.ascontiguousarray(ml[:, SEQ:].reshape(32, 4, 288))[None]]
    for g in range(3):
        dp = np.stack([np.asarray(r['dil%d_p' % g]) for r in res]).reshape(8, Ws[g], 2, 8, 128)
        ds = np.concatenate([np.asarray(r['dil%d_s' % g]) for r in res], axis=0).reshape(32, Ws[g], 2, 8, 128)
        outs += [dp, ds]
    return tuple(np.asarray(o, dtype=np.float32) for o in outs)
```
